# Optimizing a Trainium2 kernel written in Bass

```python
import math
import jax, jax.numpy as jnp
from jax import lax
import numpy as np

D_MODEL = 1024
BATCH = 16
SEQ = 2048
DEPTH = 1

D_RNN = D_MODEL
H_R = 16
DH_R = D_RNN // H_R
RG_C = 8.0
CONV_W = 4
D_M = D_MODEL
H_M = 4
DH_M = D_M // H_M
CHUNK = 128
IN_SIZES = [D_RNN, D_RNN, D_M, D_M, D_M, H_M, H_M, D_MODEL, D_MODEL]
D_IN = sum(IN_SIZES)
EPS = 1e-6

kernel_name = "hybrid_rglru_mlstm_gated_block"


def rms_norm(x, g):
    xf = x.astype(jnp.float32)
    y = xf * lax.rsqrt(jnp.mean(xf * xf, axis=-1, keepdims=True) + EPS)
    return (y * g.astype(jnp.float32)).astype(x.dtype)


def causal_dwconv(x, w, b):
    C = x.shape[-1]
    y = lax.conv_general_dilated(
        x, w[:, None, :].astype(x.dtype), window_strides=(1,), padding=[(CONV_W - 1, 0)],
        dimension_numbers=("NWC", "WIO", "NWC"), feature_group_count=C)
    return y + b.astype(x.dtype)


def headwise(x, w):
    B, S, _ = x.shape
    H, d, e = w.shape
    y = jnp.einsum("bshd,hde->bshe", x.reshape(B, S, H, d), w)
    return y.reshape(B, S, H * e)


def rg_lru(u, gate_a, gate_x, lam):
    uf = u.astype(jnp.float32)
    r = jax.nn.sigmoid(gate_a.astype(jnp.float32))
    i = jax.nn.sigmoid(gate_x.astype(jnp.float32))
    log_a = -RG_C * r * jax.nn.softplus(-lam.astype(jnp.float32))
    a = jnp.exp(log_a)
    b = jnp.sqrt(-jnp.expm1(2.0 * log_a)) * (i * uf)

    def combine(c1, c2):
        a1, b1 = c1
        a2, b2 = c2
        return a1 * a2, a2 * b1 + b2

    _, h = lax.associative_scan(combine, (a, b), axis=1)
    return h


def mlstm_chunkwise(q, k, v, i_pre, logf):
    B, H, S, d = q.shape
    nc = S // CHUNK
    to_c = lambda t: jnp.moveaxis(t.reshape(B, H, nc, CHUNK, *t.shape[3:]), 2, 0)
    qc, kc, vc, ic, fc = to_c(q), to_c(k), to_c(v), to_c(i_pre), to_c(logf)
    causal = jnp.tril(jnp.ones((CHUNK, CHUNK), dtype=bool))

    def step(carry, inp):
        C, n, m = carry
        q_, k_, v_, i_, f_ = inp
        bcum = jnp.cumsum(f_, axis=-1)
        dmat = bcum[..., :, None] - bcum[..., None, :] + i_[..., None, :]
        dmat = jnp.where(causal, dmat, -jnp.inf)
        inter = bcum + m[..., None]
        m_j = jnp.maximum(inter, jnp.max(dmat, axis=-1))
        w = jnp.exp(dmat - m_j[..., None])
        s_qk = jnp.einsum("bhjd,bhsd->bhjs", q_, k_) * w
        e_inter = jnp.exp(inter - m_j)
        num = jnp.einsum("bhjs,bhse->bhje", s_qk, v_) + \
            e_inter[..., None] * jnp.einsum("bhjd,bhde->bhje", q_, C)
        den = jnp.sum(s_qk, axis=-1) + e_inter * jnp.einsum("bhjd,bhd->bhj", q_, n)
        h = num / jnp.maximum(jnp.abs(den), jnp.exp(-m_j))[..., None]
        b_last = bcum[..., -1]
        g = b_last[..., None] - bcum + i_
        m_new = jnp.maximum(b_last + m, jnp.max(g, axis=-1))
        decay = jnp.exp(b_last + m - m_new)
        wg = jnp.exp(g - m_new[..., None])
        C_new = decay[..., None, None] * C + jnp.einsum("bhs,bhsd,bhse->bhde", wg, k_, v_)
        n_new = decay[..., None] * n + jnp.einsum("bhs,bhsd->bhd", wg, k_)
        return (C_new, n_new, m_new), h

    init = (jnp.zeros((B, H, d, d), jnp.float32), jnp.zeros((B, H, d), jnp.float32),
            jnp.zeros((B, H), jnp.float32))
    _, hs = lax.scan(step, init, (qc, kc, vc, ic, fc))
    return jnp.moveaxis(hs, 0, 2).reshape(B, H, S, d)


def hybrid_layer(x, g_pre, w_in, rg_conv_w, rg_conv_b, rg_w_a, rg_b_a, rg_w_x, rg_b_x, rg_lambda,
                 ml_conv_w, ml_conv_b, ml_w_q, ml_w_k, ml_w_v, ml_b_i, ml_b_f, ml_norm_g, ml_skip,
                 w_branch_r, w_branch_m, w_out, g_post):
    dt = x.dtype
    B, S, _ = x.shape
    xn = rms_norm(x, g_pre)
    proj = jnp.einsum("bsd,de->bse", xn, w_in)
    offs = [0]
    for sz in IN_SIZES:
        offs.append(offs[-1] + sz)
    r_x, r_z, m_x, m_z, m_o, m_i, m_f, gate_r, gate_m = [proj[..., offs[j]:offs[j + 1]] for j in range(len(IN_SIZES))]

    u = causal_dwconv(r_x, rg_conv_w, rg_conv_b)
    h_r = rg_lru(u, headwise(u, rg_w_a) + rg_b_a, headwise(u, rg_w_x) + rg_b_x, rg_lambda)
    y_r = (h_r * jax.nn.silu(r_z.astype(jnp.float32))).astype(dt)

    c = jax.nn.silu(causal_dwconv(m_x, ml_conv_w, ml_conv_b))
    heads = lambda t: t.reshape(B, S, H_M, DH_M).transpose(0, 2, 1, 3).astype(jnp.float32)
    q = heads(headwise(c, ml_w_q))
    k = heads(headwise(c, ml_w_k)) * (1.0 / math.sqrt(DH_M))
    v = heads(headwise(m_x, ml_w_v))
    i_pre = (m_i + ml_b_i).astype(jnp.float32).transpose(0, 2, 1)
    logf = jax.nn.log_sigmoid((m_f + ml_b_f).astype(jnp.float32)).transpose(0, 2, 1)
    h_m = mlstm_chunkwise(q, k, v, i_pre, logf).transpose(0, 2, 1, 3)
    h_m = jax.nn.sigmoid(m_o.astype(jnp.float32)).reshape(B, S, H_M, DH_M) * h_m
    mu = jnp.mean(h_m, axis=-1, keepdims=True)
    var = jnp.mean(jnp.square(h_m - mu), axis=-1, keepdims=True)
    h_m = (h_m - mu) * lax.rsqrt(var + EPS) * ml_norm_g.astype(jnp.float32).reshape(H_M, DH_M)
    h_m = h_m.reshape(B, S, D_M) + ml_skip.astype(jnp.float32) * c.astype(jnp.float32)
    y_m = (h_m * jax.nn.silu(m_z.astype(jnp.float32))).astype(dt)

    y = jax.nn.sigmoid(gate_r) * jnp.einsum("bsr,rd->bsd", y_r, w_branch_r) + \
        jax.nn.sigmoid(gate_m) * jnp.einsum("bsm,md->bsd", y_m, w_branch_m)
    out = jnp.einsum("bsd,de->bse", y, w_out)
    return x + rms_norm(out, g_post)


def setup_inputs(seed: int = 0) -> dict:
    key = jax.random.key(seed)
    ks = jax.random.split(key, 24)
    L = DEPTH
    nrm = lambda k, shape, scale: jax.random.normal(k, shape, jnp.float32) * scale
    u = jax.random.uniform(ks[9], (L, D_RNN), jnp.float32, minval=0.9, maxval=0.999)
    s = u ** (1.0 / RG_C)
    rg_lambda = jnp.log(s) - jnp.log1p(-s)
    ml_b_f = jnp.broadcast_to(jnp.linspace(3.0, 6.0, H_M, dtype=jnp.float32), (L, H_M)) + nrm(ks[16], (L, H_M), 0.01)
    return {
        "x": nrm(ks[0], (BATCH, SEQ, D_MODEL), 1.0),
        "g_pre": 1.0 + nrm(ks[1], (L, D_MODEL), 0.05),
        "w_in": nrm(ks[2], (L, D_MODEL, D_IN), D_MODEL ** -0.5),
        "rg_conv_w": nrm(ks[3], (L, CONV_W, D_RNN), CONV_W ** -0.5),
        "rg_conv_b": nrm(ks[4], (L, D_RNN), 0.02),
        "rg_w_a": nrm(ks[5], (L, H_R, DH_R, DH_R), DH_R ** -0.5),
        "rg_b_a": nrm(ks[6], (L, D_RNN), 0.02),
        "rg_w_x": nrm(ks[7], (L, H_R, DH_R, DH_R), DH_R ** -0.5),
        "rg_b_x": nrm(ks[8], (L, D_RNN), 0.02),
        "rg_lambda": rg_lambda,
        "ml_conv_w": nrm(ks[10], (L, CONV_W, D_M), CONV_W ** -0.5),
        "ml_conv_b": nrm(ks[11], (L, D_M), 0.02),
        "ml_w_q": nrm(ks[12], (L, H_M, DH_M, DH_M), DH_M ** -0.5),
        "ml_w_k": nrm(ks[13], (L, H_M, DH_M, DH_M), DH_M ** -0.5),
        "ml_w_v": nrm(ks[14], (L, H_M, DH_M, DH_M), DH_M ** -0.5),
        "ml_b_i": nrm(ks[15], (L, H_M), 0.1),
        "ml_b_f": ml_b_f,
        "ml_norm_g": 1.0 + nrm(ks[17], (L, D_M), 0.05),
        "ml_skip": 1.0 + nrm(ks[18], (L, D_M), 0.05),
        "w_branch_r": nrm(ks[19], (L, D_RNN, D_MODEL), D_RNN ** -0.5),
        "w_branch_m": nrm(ks[20], (L, D_M, D_MODEL), D_M ** -0.5),
        "w_out": nrm(ks[21], (L, D_MODEL, D_MODEL), D_MODEL ** -0.5),
        "g_post": 1.0 + nrm(ks[22], (L, D_MODEL), 0.05),
    }


def reference(x, g_pre, w_in, rg_conv_w, rg_conv_b, rg_w_a, rg_b_a, rg_w_x, rg_b_x, rg_lambda,
              ml_conv_w, ml_conv_b, ml_w_q, ml_w_k, ml_w_v, ml_b_i, ml_b_f, ml_norm_g, ml_skip,
              w_branch_r, w_branch_m, w_out, g_post):
    h = x
    for l in range(DEPTH):
        h = hybrid_layer(h, g_pre[l], w_in[l], rg_conv_w[l], rg_conv_b[l], rg_w_a[l], rg_b_a[l],
                         rg_w_x[l], rg_b_x[l], rg_lambda[l], ml_conv_w[l], ml_conv_b[l],
                         ml_w_q[l], ml_w_k[l], ml_w_v[l], ml_b_i[l], ml_b_f[l], ml_norm_g[l],
                         ml_skip[l], w_branch_r[l], w_branch_m[l], w_out[l], g_post[l])
    return h
```

```python
import numpy as np
from contextlib import ExitStack
import concourse.bass as bass
import concourse.mybir as mybir
from concourse.bass_utils import run_bass_kernel_spmd

F32 = mybir.dt.float32
BF16 = mybir.dt.bfloat16
ALU = mybir.AluOpType
AF = mybir.ActivationFunctionType

D = 1024
KC = 8
T = 512
NSUB = 4
H = 4
EPS = 1e-6
NUNIT = 20
NRING = 3
U_RX, U_RZ, U_MX, U_MZ, U_MO, U_GR, U_GM, U_BR, U_BM, U_WO = 0, 2, 4, 6, 8, 10, 12, 14, 16, 18
PV_GPRE, PV_RCB, PV_RBA, PV_RBX, PV_LAM, PV_MCB, PV_MNG, PV_MSK = range(8)


class _Op:
    __slots__ = ("eng", "fn", "deps", "dma", "semkey", "sigval", "signalled", "dmaval")

    def __init__(self, eng, fn, dma, semkey):
        self.eng = eng
        self.fn = fn
        self.deps = []
        self.dma = dma
        self.semkey = semkey
        self.sigval = 0
        self.signalled = False
        self.dmaval = 0


class Sched:
    ENGS = ("pe", "act", "dve", "pool", "sp")

    def __init__(self):
        self.ops = []
        self.last_writer = {}
        self.readers = {}
        self.dma_count = {}

    def add(self, eng, fn, reads=(), writes=(), dma=False, semkey=None):
        op = _Op(eng, fn, dma, semkey)
        deps = {}
        for r in reads:
            w = self.last_writer.get(r)
            if w is not None:
                deps[id(w)] = (w, True)
        for k in writes:
            w = self.last_writer.get(k)
            if w is not None and id(w) not in deps:
                deps[id(w)] = (w, False)
            for rd in self.readers.get(k, ()):
                if id(rd) not in deps:
                    deps[id(rd)] = (rd, False)
        for d, raw in deps.values():
            if d is op:
                continue
            if d.dma:
                op.deps.append(d)
            elif d.eng != eng:
                op.deps.append(d)
            elif eng != "pe" and not dma:
                op.deps.append(d)
            elif dma and not d.dma:
                op.deps.append(d)
        for k in writes:
            self.last_writer[k] = op
            self.readers[k] = []
        for r in reads:
            if r not in writes:
                self.readers.setdefault(r, []).append(op)
        if dma:
            n = self.dma_count.get(semkey, 0) + 1
            self.dma_count[semkey] = n
            op.dmaval = 16 * n
        self.ops.append(op)
        return op

    def finalize(self):
        for op in self.ops:
            for d in op.deps:
                if not d.dma:
                    d.signalled = True
        cnt = {e: 0 for e in self.ENGS}
        for op in self.ops:
            if op.signalled and not op.dma:
                cnt[op.eng] += 1
                op.sigval = cnt[op.eng]

    def emit(self, nc, es, final_dma_keys):
        self.finalize()
        esem = {e: es.enter_context(nc.semaphore("sem_" + e)) for e in self.ENGS}
        dsem = {}
        for k in self.dma_count:
            dsem[k] = es.enter_context(nc.semaphore("dsem_%d" % len(dsem)))
        block = es.enter_context(nc.Block())
        per = {e: [op for op in self.ops if op.eng == e] for e in self.ENGS}

        def run(eng_name, eobj):
            seen = {}
            for op in per[eng_name]:
                for d in op.deps:
                    if d.dma:
                        s, v = dsem[d.semkey], d.dmaval
                    else:
                        s, v = esem[d.eng], d.sigval
                    if seen.get(id(s), 0) < v:
                        eobj.wait_ge(s, v)
                        seen[id(s)] = v
                inst = op.fn(eobj)
                if op.dma:
                    inst.then_inc(dsem[op.semkey], 16)
                elif op.signalled:
                    inst.then_inc(esem[op.eng], 1)
            if eng_name == "sp":
                for k in final_dma_keys:
                    eobj.wait_ge(dsem[k], 16 * self.dma_count[k])

        @block.tensor
        def _(e):
            run("pe", e)

        @block.scalar
        def _(e):
            run("act", e)

        @block.vector
        def _(e):
            run("dve", e)

        @block.gpsimd
        def _(e):
            run("pool", e)

        @block.sync
        def _(e):
            run("sp", e)


def build(NSEQ, S):
    NTOK = NSEQ * S
    NTILE = S // T
    nc = bass.Bass("TRN2", target_bir_lowering=False)
    S_ = Sched()
    es = ExitStack()

    def dram(name, shape, dt, kind):
        return nc.dram_tensor(name, list(shape), dt, kind=kind).ap()

    x_d = dram("x", [NTOK, D], F32, "ExternalInput")
    wbig_d = dram("wbig", [NUNIT, 128, KC, 512], F32, "ExternalInput")
    wqkv_d = dram("wqkv", [3, 128, 8, 256], F32, "ExternalInput")
    wgate_d = dram("wgate", [2, 128, 8, 128], F32, "ExternalInput")
    wconv_d = dram("wconv", [128, 64, 128], F32, "ExternalInput")
    wif_d = dram("wif", [128, 8, 8], F32, "ExternalInput")
    pvec_d = dram("pvec", [128, 8, 8], F32, "ExternalInput")
    bif_d = dram("bif", [4, 2], F32, "ExternalInput")
    gpost_d = dram("gpost", [1, D], F32, "ExternalInput")
    cid_d = dram("c_ident", [128, 128], F32, "ExternalInput")
    cmask_d = dram("c_mask", [128, 128], F32, "ExternalInput")
    out_d = dram("out", [NTOK, D], F32, "ExternalOutput")
    wbf_d = dram("wbf", [NUNIT, 128, KC, 512], BF16, "Internal")

    def sb(name, shape, dt=F32):
        return es.enter_context(nc.sbuf_tensor("s_" + name, list(shape), dt))

    ring = sb("ring", [128, NRING, KC, 512], BF16)
    xt = sb("xt", [128, 2, D])
    xs = sb("xs", [128, D], BF16)
    xnT = sb("xnT", [128, KC, T], BF16)
    gTbc = sb("gTbc", [128, KC, 128])
    gpost_bc = sb("gpost_bc", [128, D])
    ident = sb("ident", [128, 128], BF16)
    ident32 = sb("ident32", [128, 128])
    maskT = sb("maskT", [128, 128])
    ones4 = sb("ones4", [4, 128])
    onesrow = sb("onesrow", [4, T])
    neghalf = sb("neghalf", [128, 4])
    eye4bc = sb("eye4bc", [4, 4, 4])
    pv = sb("pv", [128, 8, 8])
    cv1 = sb("cv1", [128, 8])
    cvh = sb("cvh", [128, 8])
    hba = sb("hba", [128, 8])
    hbx = sb("hbx", [128, 8])
    ptmp = sb("ptmp", [128, 8])
    bif = sb("bif", [4, 2])
    nbf = sb("nbf", [4, 1])
    wq = sb("wq", [128, 8, 256], BF16)
    wk = sb("wk", [128, 8, 256], BF16)
    wv = sb("wv", [128, 8, 256], BF16)
    wa = sb("wa", [128, 8, 128], BF16)
    wx = sb("wx", [128, 8, 128], BF16)
    wcv = sb("wcv", [128, 64, 128], BF16)
    wif = sb("wif", [128, 8, 8], BF16)
    rxb = sb("rxb", [128, KC, 3 + T], BF16)
    mxb = sb("mxb", [128, KC, 3 + T], BF16)
    ss = sb("ss", [128, 4])
    ms = sb("ms", [128, 4])
    rstd = sb("rstd", [128, 4])
    ubf = sb("ubf", [128, T], BF16)
    u32 = sb("u32", [128, T])
    tr = sb("tr", [128, T])
    ti = sb("ti", [128, T])
    a_t = sb("a_t", [128, T])
    a2s = sb("a2s", [128, T])
    hh = sb("hh", [128, T])
    tz = sb("tz", [128, T])
    hst = sb("hst", [128, KC])
    yrT = sb("yrT", [128, KC, T], BF16)
    ymT = sb("ymT", [128, KC, T], BF16)
    Bc = sb("Bc", [4, 1])
    Gext = sb("Gext", [4, 5])
    dl0 = sb("dl0", [4, 4])
    dl = sb("dl", [4, 4])
    rhsd = sb("rhsd", [4, 4, 4])
    wtok = sb("wtok", [128, NSUB, 8])
    dlt = sb("dlt", [128, 16])
    cbf = sb("cbf", [128, 2, T], BF16)
    scm = sb("scm", [128, 2, T])
    qT = sb("qT", [128, 2, T], BF16)
    kT = sb("kT", [128, 2, T], BF16)
    kb = sb("kb", [128, NSUB, 256], BF16)
    wva = sb("wva", [128, NSUB, 260], BF16)
    szm = sb("szm", [128, 2, T])
    to_ = sb("to_", [128, NSUB, 256])
    hmp = to_
    hn = sb("hn", [128, NSUB, 256], BF16)
    smT = sb("smT", [128, 2, 128], BF16)
    t1 = sb("t1", [128, 2, T])
    c32 = t1
    Cst = sb("Cst", [128, H + 1, 2, 260])
    Cbf = sb("Cbf", [128, 2, 2, 260], BF16)
    stats = sb("stats", [128, NSUB, 6])
    mv = sb("mv", [128, NSUB, 2])
    den = sb("den", [128, NSUB])
    epsv = sb("epsv", [128, NSUB])
    rs4 = sb("rs4", [128, NSUB])
    nmr = sb("nmr", [128, NSUB])
    trm = sb("trm", [128, 2, T])
    y1 = sb("y1", [128, 4, T])
    y2 = sb("y2", [128, T])
    yT = sb("yT", [128, KC, T], BF16)
    obuf = sb("obuf", [128, 1, D])
    ss2 = sb("ss2", [128, 2, 2])
    ms2 = sb("ms2", [128, 2])
    rs2 = sb("rs2", [128, 2])

    ps = es.enter_context(nc.psum_tensor("ps", [128, 8, 512], F32))

    def bank(b):
        return ps[:, b, :]

    def bank_bf(b):
        return ps[:, b, :].bitcast(BF16)

    _bk = [0]

    def nb():
        b = _bk[0]
        _bk[0] = (b + 1) % 8
        return b

    def mm(out, lhsT, rhs, start, stop, reads, writes):
        S_.add("pe", lambda e: e.matmul(out, lhsT, rhs, start=start, stop=stop), reads, writes)

    def tp(out, in_, idn, reads, writes):
        S_.add("pe", lambda e: e.transpose(out, in_, idn), reads, writes)

    def act(out, in_, func, reads, writes, bias=None, scale=None, accum=None):
        kw = {}
        if bias is not None:
            kw["bias"] = bias
        if scale is not None:
            kw["scale"] = scale
        if accum is not None:
            kw["accum_out"] = accum
        S_.add("act", lambda e: e.activation(out, in_, func, **kw), reads, writes)

    def tt(eng, out, in0, in1, op, reads, writes):
        S_.add(eng, lambda e: e.tensor_tensor(out, in0, in1, op), reads, writes)

    def ts(eng, out, in0, s1, s2, op0, op1, reads, writes):
        if s2 is None:
            S_.add(eng, lambda e: e.tensor_scalar(out, in0, s1, None, op0), reads, writes)
        else:
            S_.add(eng, lambda e: e.tensor_scalar(out, in0, s1, s2, op0, op1), reads, writes)

    def stt(out, in0, scalar, in1, op0, op1, reads, writes):
        S_.add("dve", lambda e: e.scalar_tensor_tensor(out, in0, scalar, in1, op0, op1), reads, writes)

    def scan(out, d0, d1, init, op0, op1, reads, writes):
        S_.add("dve", lambda e: e.tensor_tensor_scan(out, d0, d1, init, op0, op1), reads, writes)

    def cp(eng, out, in_, reads, writes):
        S_.add(eng, lambda e: e.tensor_copy(out, in_), reads, writes)

    def dma(eng, out, in_, reads, writes, semkey, **kw):
        S_.add(eng, lambda e: e.dma_start(out=out, in_=in_, **kw), reads, writes, dma=True, semkey=semkey)

    def memset(eng, ap, val, writes):
        S_.add(eng, lambda e: e.memset(ap, val), (), writes)

    for g in range(NUNIT):
        dma("pool", wbf_d[g], wbig_d[g], (), [("wbf", g)], ("wbf", g), max_dma_last_dim=4096)
    dma("pool", wq[:], wqkv_d[0], (), ["wq"], "ld_wq", max_dma_last_dim=4096)
    dma("pool", wk[:], wqkv_d[1], (), ["wk"], "ld_wk", max_dma_last_dim=4096)
    dma("pool", wv[:], wqkv_d[2], (), ["wv"], "ld_wv", max_dma_last_dim=4096)
    dma("pool", wa[:], wgate_d[0], (), ["wa"], "ld_wa", max_dma_last_dim=4096)
    dma("pool", wx[:], wgate_d[1], (), ["wx"], "ld_wx", max_dma_last_dim=4096)
    dma("pool", wcv[:], wconv_d, (), ["wcv"], "ld_wcv", max_dma_last_dim=4096)
    dma("pool", wif[:], wif_d, (), ["wif"], "ld_wif")
    dma("pool", ident[:], cid_d, (), ["ident"], "ld_ident")
    dma("sp", ident32[:], cid_d, (), ["ident32"], "ld_c1")
    dma("sp", maskT[:], cmask_d, (), ["maskT"], "ld_c2")
    dma("sp", pv[:], pvec_d, (), ["pv"], "ld_c3")
    dma("sp", bif[:], bif_d, (), ["bif"], "ld_c4")
    dma("sp", gpost_bc[:], gpost_d.partition_broadcast(128), (), ["gpost_bc"], "ld_c5")
    memset("pool", ones4[:], 1.0, ["ones4"])
    memset("pool", onesrow[:], 1.0, ["onesrow"])
    memset("pool", neghalf[:], -0.5, ["neghalf"])
    memset("pool", wva[:], 0.0, ["wva"])
    memset("pool", Cbf[:], 0.0, ["Cbf"])
    memset("pool", rxb[:], 0.0, [("rxb", c) for c in range(KC)])
    memset("pool", mxb[:], 0.0, [("mxb", c) for c in range(KC)])
    for c in range(4):
        cp("dve", eye4bc[:, c, :], ident32[0:4, 0:4], ["ident32"], ["eye4bc"])
    for k in range(KC):
        cp("dve", gTbc[:, k, :], pv[:, PV_GPRE, k:k + 1].to_broadcast([128, 128]), ["pv"], ["gTbc"])
    act(ptmp[:], pv[:, PV_LAM, :], AF.Exp, ["pv"], ["ptmp"], scale=-1.0)
    act(ptmp[:], ptmp[:], AF.Ln, ["ptmp"], ["ptmp"], bias=1.0)
    ts("dve", cv1[:], ptmp[:], -8.0, None, ALU.mult, None, ["ptmp"], ["cv1"])
    ts("dve", cvh[:], ptmp[:], -4.0, None, ALU.mult, None, ["ptmp"], ["cvh"])
    ts("dve", hba[:], pv[:, PV_RBA, :], 0.5, None, ALU.mult, None, ["pv"], ["hba"])
    ts("dve", hbx[:], pv[:, PV_RBX, :], 0.5, None, ALU.mult, None, ["pv"], ["hbx"])
    ts("dve", nbf[:], bif[:, 1:2], -1.0, None, ALU.mult, None, ["bif"], ["nbf"])

    ring_fill = [0]

    def load_unit(u):
        slot = ring_fill[0] % NRING
        ring_fill[0] += 1
        dma("sp", ring[:, slot], wbf_d[u], [("wbf", u)], [("ring", slot)], ("ring", slot))
        return slot

    def lhs_unit(slot, k, j):
        return ring[:, slot, k, j * 128:(j + 1) * 128]

    def proj_fm(slot, j, b, rkeys=()):
        for k in range(KC):
            mm(bank(b), lhs_unit(slot, k, j), xnT[:, k, :], k == 0, k == KC - 1,
               [("ring", slot), "xnT"] + list(rkeys), [("ps", b)])

    out_keys = []

    for tile in range(NSEQ * NTILE):
        seq, tl = divmod(tile, NTILE)
        first = tl == 0
        tok0 = seq * S + tl * T

        for sub in range(NSUB):
            slot = sub % 2
            r0 = tok0 + sub * 128
            dma("sp", xt[:, slot, :], x_d[r0:r0 + 128, :], (), [("xt", slot)], ("xt", slot))
            act(xs[:], xt[:, slot, :], AF.Square, [("xt", slot)], ["xs", ("ss", sub)],
                accum=ss[:, sub:sub + 1])
            ts("dve", ms[:, sub:sub + 1], ss[:, sub:sub + 1], 1.0 / D, EPS, ALU.mult, ALU.add,
               [("ss", sub)], [("ms", sub)])
            tt("pool", rstd[:, sub:sub + 1], ms[:, sub:sub + 1], neghalf[:, 0:1], ALU.pow,
               [("ms", sub), "neghalf"], [("rstd", sub)])
            act(xs[:], xt[:, slot, :], AF.Copy, [("xt", slot), ("rstd", sub)], ["xs"],
                scale=rstd[:, sub:sub + 1])
            b = nb()
            for k in range(KC):
                tp(bank_bf(b)[:, k * 128:(k + 1) * 128], xs[:, k * 128:(k + 1) * 128], ident[:],
                   ["xs", "ident"], [("ps", b)])
            tt("dve", xnT[:, :, sub * 128:(sub + 1) * 128],
               bank_bf(b).rearrange("p (k t) -> p k t", k=KC), gTbc[:], ALU.mult,
               [("ps", b), "gTbc"], ["xnT"])

        bi_, bf_ = nb(), nb()
        for k in range(KC):
            mm(bank(bi_)[0:4, :], wif[:, k, 0:4], xnT[:, k, :], k == 0, k == KC - 1,
               ["wif", "xnT"], [("ps", bi_)])
        for k in range(KC):
            mm(bank(bf_)[0:4, :], wif[:, k, 4:8], xnT[:, k, :], k == 0, k == KC - 1,
               ["wif", "xnT"], [("ps", bf_)])
        if first:
            memset("dve", Bc[:], 0.0, ["Bc"])
            memset("dve", Gext[:], 0.0, ["Gext"])
            memset("dve", hst[:], 0.0, ["hst"])
            memset("dve", Cst[:], 0.0, [("Cst", h_) for h_ in range(H)] + ["Ctmp"])
            memset("pool", rxb[:, :, 0:3], 0.0, [("rxb", c) for c in range(KC)])
            memset("pool", mxb[:, :, 0:3], 0.0, [("mxb", c) for c in range(KC)])
        else:
            cp("dve", Gext[:, 0:1], Gext[:, 4:5], ["Gext"], ["Gext"])
        lfv, Btv, Atv, Gtv, E1v, E2v = u32[0:4, :], tr[0:4, :], ti[0:4, :], a_t[0:4, :], a2s[0:4, :], tz[0:4, :]
        c3 = "p (c t) -> p c t"
        act(lfv, bank(bf_)[0:4, :], AF.Exp, [("ps", bf_), "nbf"], ["u32"], bias=nbf[:, 0:1], scale=-1.0)
        act(lfv, lfv, AF.Ln, ["u32"], ["u32"], bias=1.0)
        scan(Btv, onesrow[:], lfv, Bc[:, 0:1], ALU.mult, ALU.subtract, ["onesrow", "u32", "Bc"], ["tr"])
        cp("dve", Bc[:], Btv[:, T - 1:T], ["tr"], ["Bc"])
        stt(Atv, bank(bi_)[0:4, :], bif[:, 0:1], Btv, ALU.add, ALU.subtract,
            [("ps", bi_), "bif", "tr"], ["ti"])
        scan(Gtv, Atv, Atv, Gext[:, 0:1], ALU.max, ALU.max, ["ti", "Gext"], ["a_t"])
        Gv = Gtv.rearrange(c3, t=128)
        cp("dve", Gext[:, 1:5].unsqueeze(2), Gv[:, :, 127:128], ["a_t"], ["Gext"])
        tt("dve", dl0[:], Gext[:, 0:4], Gext[:, 1:5], ALU.subtract, ["Gext"], ["dl0"])
        act(dl[:], dl0[:], AF.Exp, ["dl0"], ["dl"])
        Rb = Gext[:, 1:5].unsqueeze(2).to_broadcast([4, 4, 128])
        tt("dve", E1v.rearrange(c3, t=128), Atv.rearrange(c3, t=128), Rb, ALU.subtract, ["ti", "Gext"], ["a2s"])
        act(E1v, E1v, AF.Exp, ["a2s"], ["a2s"])
        tt("dve", E2v.rearrange(c3, t=128), Btv.rearrange(c3, t=128), Rb, ALU.add, ["tr", "Gext"], ["tz"])
        act(E2v, E2v, AF.Exp, ["tz"], ["tz"], scale=-1.0)
        bt_ = nb()
        for c in range(NSUB):
            mm(bank(bt_)[:, c * 8:c * 8 + 4], E1v[:, c * 128:(c + 1) * 128], ident32[0:4, 0:4], True, True,
               ["a2s", "ident32"], [("ps", bt_)])
            mm(bank(bt_)[:, c * 8 + 4:c * 8 + 8], E2v[:, c * 128:(c + 1) * 128], ident32[0:4, 0:4], True, True,
               ["tz", "ident32"], [("ps", bt_)])
        cp("dve", wtok[:].rearrange("p c j -> p (c j)"), bank(bt_)[:, 0:32], [("ps", bt_)], ["wtok"])
        tt("dve", rhsd[:], dl[:].unsqueeze(2).to_broadcast([4, 4, 4]), eye4bc[:], ALU.mult,
           ["dl", "eye4bc"], ["rhsd"])
        bd_ = nb()
        mm(bank(bd_)[:, 0:16], ones4[:], rhsd[:].rearrange("p c h -> p (c h)"), True, True,
           ["ones4", "rhsd"], [("ps", bd_)])
        cp("dve", dlt[:], bank(bd_)[:, 0:16], [("ps", bd_)], ["dlt"])

        slot_rx = slot_rz = None
        for c in range(KC):
            j = c % 4
            if j == 0:
                slot_rx = load_unit(U_RX + c // 4)
                slot_rz = load_unit(U_RZ + c // 4)
            b_rx, b_rz = nb(), nb()
            proj_fm(slot_rx, j, b_rx)
            proj_fm(slot_rz, j, b_rz)
            act(rxb[:, c, 3:3 + T], bank(b_rx), AF.Copy, [("ps", b_rx)], [("rxb", c)])
            b_u = nb()
            for jj in range(4):
                mm(bank(b_u), wcv[:, c * 4 + jj, :], rxb[:, c, jj:jj + T], jj == 0, jj == 3,
                   ["wcv", ("rxb", c)], [("ps", b_u)])
            act(ubf[:], bank(b_u), AF.Identity, [("ps", b_u), "pv"], ["ubf"], bias=pv[:, PV_RCB, c:c + 1])
            act(u32[:], bank(b_u), AF.Identity, [("ps", b_u), "pv"], ["u32"], bias=pv[:, PV_RCB, c:c + 1])
            cp("pool", rxb[:, c, 0:3], rxb[:, c, T:T + 3], [("rxb", c)], [("rxb", c)])
            b_a, b_x = nb(), nb()
            mm(bank(b_a), wa[:, c, :], ubf[:], True, True, ["wa", "ubf"], [("ps", b_a)])
            mm(bank(b_x), wx[:, c, :], ubf[:], True, True, ["wx", "ubf"], [("ps", b_x)])
            act(tr[:], bank(b_a), AF.Tanh, [("ps", b_a), "hba"], ["tr"], bias=hba[:, c:c + 1], scale=0.5)
            act(ti[:], bank(b_x), AF.Tanh, [("ps", b_x), "hbx"], ["ti"], bias=hbx[:, c:c + 1], scale=0.5)
            act(tz[:], bank(b_rz), AF.Tanh, [("ps", b_rz)], ["tz"], scale=0.5)
            act(a_t[:], tr[:], AF.Exp, ["tr", "cvh"], ["a_t"], bias=cvh[:, c:c + 1], scale=cvh[:, c:c + 1])
            act(a2s[:], tr[:], AF.Exp, ["tr", "cv1"], ["a2s"], bias=cv1[:, c:c + 1], scale=cv1[:, c:c + 1])
            act(a2s[:], a2s[:], AF.Sqrt, ["a2s"], ["a2s"], bias=1.0, scale=-1.0)
            stt(u32[:], ti[:], 1.0, u32[:], ALU.add, ALU.mult, ["ti", "u32"], ["u32"])
            tt("dve", a2s[:], a2s[:], u32[:], ALU.mult, ["a2s", "u32"], ["a2s"])
            scan(hh[:], a_t[:], a2s[:], hst[:, c:c + 1], ALU.mult, ALU.add, ["a_t", "a2s", "hst"], ["hh"])
            cp("dve", hst[:, c:c + 1], hh[:, T - 1:T], ["hh"], ["hst"])
            stt(hh[:], bank(b_rz), 0.25, hh[:], ALU.mult, ALU.mult, [("ps", b_rz), "hh"], ["hh"])
            stt(yrT[:, c, :], tz[:], 1.0, hh[:], ALU.add, ALU.mult, ["tz", "hh"], [("yrT", c)])

        slot_mx = slot_mz = slot_mo = None
        for h_ in range(H):
            if h_ % 2 == 0:
                slot_mx = load_unit(U_MX + h_ // 2)
                slot_mz = load_unit(U_MZ + h_ // 2)
                slot_mo = load_unit(U_MO + h_ // 2)
            for e2 in range(2):
                c = 2 * h_ + e2
                j = c % 4
                b_mx = nb()
                proj_fm(slot_mx, j, b_mx)
                act(mxb[:, c, 3:3 + T], bank(b_mx), AF.Copy, [("ps", b_mx)], [("mxb", c)])
                b_c = nb()
                for jj in range(4):
                    mm(bank(b_c), wcv[:, 32 + c * 4 + jj, :], mxb[:, c, jj:jj + T], jj == 0, jj == 3,
                       ["wcv", ("mxb", c)], [("ps", b_c)])
                act(c32[:, e2, :], bank(b_c), AF.Silu, [("ps", b_c), "pv"], [("t1", e2)],
                    bias=pv[:, PV_MCB, c:c + 1])
                act(cbf[:, e2, :], c32[:, e2, :], AF.Copy, [("t1", e2)], [("cbf", e2)])
                act(scm[:, e2, :], c32[:, e2, :], AF.Copy, [("t1", e2), "pv"], [("scm", e2)],
                    scale=pv[:, PV_MSK, c:c + 1])
                b_mz = nb()
                proj_fm(slot_mz, j, b_mz)
                act(szm[:, e2, :], bank(b_mz), AF.Silu, [("ps", b_mz)], [("szm", e2)])
            for e2 in range(2):
                b_q = nb()
                for kc in range(2):
                    mm(bank(b_q), wq[:, h_ * 2 + kc, e2 * 128:(e2 + 1) * 128], cbf[:, kc, :], kc == 0, kc == 1,
                       ["wq", ("cbf", 0), ("cbf", 1)], [("ps", b_q)])
                act(qT[:, e2, :], bank(b_q), AF.Copy, [("ps", b_q)], [("qT", e2)])
                b_k = nb()
                for kc in range(2):
                    mm(bank(b_k), wk[:, h_ * 2 + kc, e2 * 128:(e2 + 1) * 128], cbf[:, kc, :], kc == 0, kc == 1,
                       ["wk", ("cbf", 0), ("cbf", 1)], [("ps", b_k)])
                act(kT[:, e2, :], bank(b_k), AF.Copy, [("ps", b_k)], [("kT", e2)], scale=1.0 / 16.0)
            cp("dve", wva[:, :, 256], wtok[:, :, h_], ["wtok"], ["wva"])
            for sub in range(NSUB):
                tsl = slice(sub * 128, (sub + 1) * 128)
                b_kv = nb()
                for kc in range(2):
                    mm(bank(b_kv)[:, 0:256], cbf[:, kc, tsl], wk[:, h_ * 2 + kc, :], kc == 0, kc == 1,
                       ["wk", ("cbf", 0), ("cbf", 1)], [("ps", b_kv)])
                for kc in range(2):
                    mm(bank(b_kv)[:, 256:512], mxb[:, 2 * h_ + kc, 3 + sub * 128:3 + (sub + 1) * 128],
                       wv[:, h_ * 2 + kc, :], kc == 0, kc == 1,
                       ["wv", ("mxb", 2 * h_), ("mxb", 2 * h_ + 1)], [("ps", b_kv)])
                act(kb[:, sub, :], bank(b_kv)[:, 0:256], AF.Copy, [("ps", b_kv)], ["kb"], scale=1.0 / 16.0)
                act(wva[:, sub, 0:256], bank(b_kv)[:, 256:512], AF.Copy, [("ps", b_kv), "wtok"], ["wva"],
                    scale=wtok[:, sub, h_:h_ + 1])
                b_o = nb()
                hh2 = h_ % 2
                for k in range(KC):
                    mm(bank(b_o)[:, 0:256], xnT[:, k, tsl], ring[:, slot_mo, k, hh2 * 256:(hh2 + 1) * 256],
                       k == 0, k == KC - 1, ["xnT", ("ring", slot_mo)], [("ps", b_o)])
                act(to_[:, sub, :], bank(b_o)[:, 0:256], AF.Tanh, [("ps", b_o)], [("to_", sub)], scale=0.5)
            for e2 in range(2):
                c = 2 * h_ + e2
                cp("pool", mxb[:, c, 0:3], mxb[:, c, T:T + 3], [("mxb", c)], [("mxb", c)])
            for sub in range(NSUB):
                tsl = slice(sub * 128, (sub + 1) * 128)
                par = sub % 2
                cur = Cst[:, h_ if par == 0 else H, :, 0:257]
                nxt = Cst[:, H if par == 0 else h_, :, 0:257]
                dsc = dlt[:, sub * 4 + h_:sub * 4 + h_ + 1]
                act(Cbf[:, par, :, 0:257], cur, AF.Copy, [("Cst", h_), "Ctmp", "dlt"], [("Cbf", par)], scale=dsc)
                b_s = nb()
                for kc in range(2):
                    mm(bank(b_s)[:, 0:128], kT[:, kc, tsl], qT[:, kc, tsl], kc == 0, kc == 1,
                       [("kT", 0), ("kT", 1), ("qT", 0), ("qT", 1)], [("ps", b_s)])
                tt("dve", smT[:, par, :], bank(b_s)[:, 0:128], maskT[:], ALU.mult,
                   [("ps", b_s), "maskT"], [("smT", par)])
                b_p = nb()
                mm(bank(b_p)[:, 0:257], smT[:, par, :], wva[:, sub, 0:257], True, False,
                   [("smT", par), "wva"], [("ps", b_p)])
                for kc in range(2):
                    mm(bank(b_p)[:, 0:257], qT[:, kc, tsl], Cbf[:, par, kc, 0:257], False, kc == 1,
                       [("qT", 0), ("qT", 1), ("Cbf", par)], [("ps", b_p)])
                b_d0, b_d1 = nb(), nb()
                for kc, bdd in ((0, b_d0), (1, b_d1)):
                    mm(bank(bdd)[:, 0:257], kb[:, sub, kc * 128:(kc + 1) * 128], wva[:, sub, 0:257], True, True,
                       ["kb", "wva"], [("ps", bdd)])
                for kc, bdd in ((0, b_d0), (1, b_d1)):
                    stt(nxt[:, kc, :], cur[:, kc, :], dsc, bank(bdd)[:, 0:257], ALU.mult, ALU.add,
                        [("Cst", h_), "Ctmp", "dlt", ("ps", bdd)], [("Cst", h_), "Ctmp"])
                stt(hmp[:, sub, :], to_[:, sub, :], 1.0, bank(b_p)[:, 0:256], ALU.add, ALU.mult,
                    [("to_", sub), ("ps", b_p)], [("to_", sub)])
                ts("dve", den[:, sub:sub + 1], bank(b_p)[:, 256:257], -1.0, wtok[:, sub, 4 + h_:5 + h_],
                   ALU.mult, ALU.max, [("ps", b_p), "wtok"], [("den", sub)])
                tt("dve", den[:, sub:sub + 1], den[:, sub:sub + 1], bank(b_p)[:, 256:257], ALU.max,
                   [("ps", b_p), ("den", sub)], [("den", sub)])
                S_.add("dve", (lambda o, i: (lambda e: e.bn_stats(o, i)))(stats[:, sub, :], hmp[:, sub, :]),
                       [("to_", sub)], [("stats", sub)])
                S_.add("dve", (lambda o, i: (lambda e: e.bn_aggr(o, i)))(mv[:, sub, :], stats[:, sub, :]),
                       [("stats", sub)], [("mv", sub)])
            allsub = list(range(NSUB))
            tt("dve", epsv[:], den[:], den[:], ALU.mult, [("den", s) for s in allsub], ["epsv"])
            stt(epsv[:], epsv[:], 4.0 * EPS, mv[:, :, 1], ALU.mult, ALU.add,
                ["epsv"] + [("mv", s) for s in allsub], ["epsv"])
            tt("pool", rs4[:], epsv[:], neghalf[:], ALU.pow, ["epsv", "neghalf"], ["rs4"])
            stt(nmr[:], mv[:, :, 0], -1.0, rs4[:], ALU.mult, ALU.mult, [("mv", s) for s in allsub] + ["rs4"], ["nmr"])
            b_t = nb()
            for sub in range(NSUB):
                act(hn[:, sub, :], hmp[:, sub, :], AF.Identity, [("to_", sub), "rs4", "nmr"], [("hn", sub)],
                    bias=nmr[:, sub:sub + 1], scale=rs4[:, sub:sub + 1])
                for e2 in range(2):
                    tp(bank_bf(b_t)[:, e2 * 512 + sub * 128:e2 * 512 + (sub + 1) * 128],
                       hn[:, sub, e2 * 128:(e2 + 1) * 128], ident[:], [("hn", sub), "ident"], [("ps", b_t)])
            for e2 in range(2):
                c = 2 * h_ + e2
                stt(t1[:, e2, :], bank_bf(b_t)[:, e2 * 512:(e2 + 1) * 512], pv[:, PV_MNG, c:c + 1], scm[:, e2, :],
                    ALU.mult, ALU.add, [("ps", b_t), "pv", ("scm", e2)], [("t1", e2)])
                tt("dve", ymT[:, c, :], t1[:, e2, :], szm[:, e2, :], ALU.mult, [("t1", e2), ("szm", e2)], [("ymT", c)])

        for half in range(2):
            s_gr = load_unit(U_GR + half)
            s_br = load_unit(U_BR + half)
            for j in range(4):
                dc = half * 4 + j
                b_g = nb()
                proj_fm(s_gr, j, b_g)
                act(trm[:, j % 2, :], bank(b_g), AF.Tanh, [("ps", b_g)], [("trm", j % 2)], scale=0.5)
                b_r = nb()
                for k in range(KC):
                    mm(bank(b_r), lhs_unit(s_br, k, j), yrT[:, k, :], k == 0, k == KC - 1,
                       [("ring", s_br)] + [("yrT", k)], [("ps", b_r)])
                stt(y1[:, j, :], trm[:, j % 2, :], 1.0, bank(b_r), ALU.add, ALU.mult,
                    [("trm", j % 2), ("ps", b_r)], [("y1", j)])
            s_gm = load_unit(U_GM + half)
            s_bm = load_unit(U_BM + half)
            for j in range(4):
                dc = half * 4 + j
                b_g = nb()
                proj_fm(s_gm, j, b_g)
                act(trm[:, j % 2, :], bank(b_g), AF.Tanh, [("ps", b_g)], [("trm", j % 2)], scale=0.5)
                b_m = nb()
                for k in range(KC):
                    mm(bank(b_m), lhs_unit(s_bm, k, j), ymT[:, k, :], k == 0, k == KC - 1,
                       [("ring", s_bm)] + [("ymT", k)], [("ps", b_m)])
                stt(y2[:], trm[:, j % 2, :], 1.0, bank(b_m), ALU.add, ALU.mult,
                    [("trm", j % 2), ("ps", b_m)], ["y2"])
                tt("dve", yT[:, dc, :], y1[:, j, :], y2[:], ALU.add, [("y1", j), "y2"], [("yT", dc)])
        s_o0 = load_unit(U_WO)
        s_o1 = load_unit(U_WO + 1)
        for sub in range(NSUB):
            tsl = slice(sub * 128, (sub + 1) * 128)
            osl = sub % 2
            r0 = tok0 + sub * 128
            bo = [nb(), nb()]
            for hf, so in ((0, s_o0), (1, s_o1)):
                for k in range(KC):
                    mm(bank(bo[hf]), yT[:, k, tsl], ring[:, so, k, :], k == 0, k == KC - 1,
                       [("yT", k), ("ring", so)], [("ps", bo[hf])])
                act(xs[:, 0:512], bank(bo[hf]), AF.Square, [("ps", bo[hf])], ["xs", ("ss2", osl, hf)],
                    accum=ss2[:, osl, hf:hf + 1])
            tt("dve", ms2[:, osl:osl + 1], ss2[:, osl, 0:1], ss2[:, osl, 1:2], ALU.add,
               [("ss2", osl, 0), ("ss2", osl, 1)], [("ms2", osl)])
            ts("dve", ms2[:, osl:osl + 1], ms2[:, osl:osl + 1], 1.0 / D, 4.0 * EPS, ALU.mult, ALU.add,
               [("ms2", osl)], [("ms2", osl)])
            tt("pool", rs2[:, osl:osl + 1], ms2[:, osl:osl + 1], neghalf[:, 0:1], ALU.pow,
               [("ms2", osl), "neghalf"], [("rs2", osl)])
            dma("sp", xt[:, osl, :], x_d[r0:r0 + 128, :], (), [("xt", osl)], ("xt", osl))
            for hf in range(2):
                stt(obuf[:, 0, hf * 512:(hf + 1) * 512], bank(bo[hf]), rs2[:, osl:osl + 1],
                    gpost_bc[:, hf * 512:(hf + 1) * 512], ALU.mult, ALU.mult,
                    [("ps", bo[hf]), ("rs2", osl), "gpost_bc"], ["obuf"])
            tt("pool", obuf[:, 0, :], obuf[:, 0, :], xt[:, osl, :], ALU.add,
               ["obuf", ("xt", osl)], ["obuf"])
            dma("sp", out_d[r0:r0 + 128, :], obuf[:, 0, :], ["obuf"], [("out", tile, sub)], ("st", 0))
    out_keys = [("st", 0)]
    S_.emit(nc, es, out_keys)
    es.close()
    return nc


def prep_weights(inp):
    f = np.float32
    w_in = np.asarray(inp["w_in"][0], f)
    cols = {}
    offs = [0, 1024, 2048, 3072, 4096, 5120, 5124, 5128, 6152, 7176]
    names = ["r_x", "r_z", "m_x", "m_z", "m_o", "m_i", "m_f", "gate_r", "gate_m"]
    for n, a, b in zip(names, offs[:-1], offs[1:]):
        cols[n] = w_in[:, a:b]
    big = [cols["r_x"], cols["r_z"], cols["m_x"], cols["m_z"], cols["m_o"], cols["gate_r"], cols["gate_m"],
           np.asarray(inp["w_branch_r"][0], f), np.asarray(inp["w_branch_m"][0], f), np.asarray(inp["w_out"][0], f)]
    units = []
    for W in big:
        for hf in range(2):
            Wh = W[:, hf * 512:(hf + 1) * 512]
            units.append(Wh.reshape(8, 128, 512).transpose(1, 0, 2))
    wbig = np.ascontiguousarray(np.stack(units, 0))
    qkv = []
    for n in ("ml_w_q", "ml_w_k", "ml_w_v"):
        W = np.asarray(inp[n][0], f)
        qkv.append(W.reshape(4, 2, 128, 256).transpose(2, 0, 1, 3).reshape(128, 8, 256))
    wqkv = np.ascontiguousarray(np.stack(qkv, 0))
    gates = []
    for n in ("rg_w_a", "rg_w_x"):
        W = np.asarray(inp[n][0], f)
        G = np.zeros((128, 8, 128), f)
        for c in range(8):
            G[0:64, c, 0:64] = W[2 * c]
            G[64:128, c, 64:128] = W[2 * c + 1]
        gates.append(G)
    wgate = np.ascontiguousarray(np.stack(gates, 0))
    wconv = np.zeros((128, 64, 128), f)
    ar = np.arange(128)
    for cv, n in enumerate(("rg_conv_w", "ml_conv_w")):
        W = np.asarray(inp[n][0], f)
        for c in range(8):
            for j in range(4):
                wconv[ar, (cv * 8 + c) * 4 + j, ar] = W[j, c * 128:(c + 1) * 128]
    wif = np.ascontiguousarray(w_in[:, 5120:5128].reshape(8, 128, 8).transpose(1, 0, 2))
    pnames = ["g_pre", "rg_conv_b", "rg_b_a", "rg_b_x", "rg_lambda", "ml_conv_b", "ml_norm_g", "ml_skip"]
    pvec = np.ascontiguousarray(
        np.stack([np.asarray(inp[n][0], f).reshape(8, 128).T for n in pnames], 1))
    bif = np.ascontiguousarray(np.stack([np.asarray(inp["ml_b_i"][0], f), np.asarray(inp["ml_b_f"][0], f)], 1))
    gpost = np.ascontiguousarray(np.asarray(inp["g_post"][0], f).reshape(1, 1024))
    c_ident = np.eye(128, dtype=f)
    c_mask = np.triu(np.ones((128, 128), f))
    return dict(wbig=wbig, wqkv=wqkv, wgate=wgate, wconv=wconv, wif=wif, pvec=pvec, bif=bif, gpost=gpost,
                c_ident=c_ident, c_mask=c_mask)


_NC_CACHE = {}


def kernel(**inputs):
    x = np.asarray(inputs["x"], np.float32)
    B, S, _ = x.shape
    ncores = 8
    nseq = B // ncores
    key = (nseq, S)
    if key not in _NC_CACHE:
        _NC_CACHE[key] = build(nseq, S)
    nc = _NC_CACHE[key]
    wts = prep_weights(inputs)
    in_maps = []
    for c in range(ncores):
        m = dict(wts)
        m["x"] = np.ascontiguousarray(x[c * nseq:(c + 1) * nseq].reshape(nseq * S, D))
        in_maps.append(m)
    res = run_bass_kernel_spmd(nc, in_maps, core_ids=list(range(ncores)))
    outs = [np.asarray(r["out"], np.float32).reshape(nseq, S, D) for r in res.results]
    return np.concatenate(outs, 0)
```

```python
import numpy as np
from contextlib import ExitStack
import concourse.bass as bass
import concourse.mybir as mybir
from concourse.bass_utils import run_bass_kernel_spmd

F32 = mybir.dt.float32
BF16 = mybir.dt.bfloat16
ALU = mybir.AluOpType
AF = mybir.ActivationFunctionType

D = 1024
KC = 8
T = 512
NSUB = 4
H = 4
EPS = 1e-6
NHU = 40
NRING = 9
HU_RX, HU_RZ, HU_MX, HU_MZ, HU_MO, HU_GR, HU_GM, HU_BR, HU_BM, HU_WO = 0, 4, 8, 12, 16, 20, 24, 28, 32, 36
PV_GPRE, PV_RCB, PV_RBA, PV_RBX, PV_LAM, PV_MCB, PV_MNG, PV_MSK = range(8)
PV_RCW = 8
PV_MCW = 12
NPV = 16


class _Op:
    __slots__ = ("eng", "fn", "deps", "dma", "semkey", "sigval", "signalled", "dmaval")

    def __init__(self, eng, fn, dma, semkey):
        self.eng = eng
        self.fn = fn
        self.deps = []
        self.dma = dma
        self.semkey = semkey
        self.sigval = 0
        self.signalled = False
        self.dmaval = 0


class Sched:
    ENGS = ("pe", "act", "dve", "pool", "sp")

    def __init__(self):
        self.ops = []
        self.last_writer = {}
        self.readers = {}
        self.dma_count = {}

    def add(self, eng, fn, reads=(), writes=(), dma=False, semkey=None):
        op = _Op(eng, fn, dma, semkey)
        deps = {}
        for r in reads:
            w = self.last_writer.get(r)
            if w is not None:
                deps[id(w)] = (w, True)
            if isinstance(r, tuple) and r[0] == "ps":
                for rd in self.readers.get(r, ()):
                    if rd.eng != eng and id(rd) not in deps:
                        deps[id(rd)] = (rd, False)
        for k in writes:
            w = self.last_writer.get(k)
            if w is not None and id(w) not in deps:
                deps[id(w)] = (w, False)
            for rd in self.readers.get(k, ()):
                if id(rd) not in deps:
                    deps[id(rd)] = (rd, False)
        for d, raw in deps.values():
            if d is op:
                continue
            if d.dma:
                op.deps.append(d)
            elif d.eng != eng:
                op.deps.append(d)
            elif eng != "pe" and not dma:
                op.deps.append(d)
            elif dma and not d.dma:
                op.deps.append(d)
        for k in writes:
            self.last_writer[k] = op
            self.readers[k] = []
        for r in reads:
            if r not in writes:
                self.readers.setdefault(r, []).append(op)
        if dma:
            n = self.dma_count.get(semkey, 0) + 1
            self.dma_count[semkey] = n
            op.dmaval = 16 * n
        self.ops.append(op)
        return op

    def finalize(self):
        for op in self.ops:
            for d in op.deps:
                if not d.dma:
                    d.signalled = True
        cnt = {e: 0 for e in self.ENGS}
        for op in self.ops:
            if op.signalled and not op.dma:
                cnt[op.eng] += 1
                op.sigval = cnt[op.eng]

    def emit(self, nc, es, final_dma_keys):
        self.finalize()
        esem = {e: es.enter_context(nc.semaphore("sem_" + e)) for e in self.ENGS}
        dsem = {}
        for k in self.dma_count:
            dsem[k] = es.enter_context(nc.semaphore("dsem_%d" % len(dsem)))
        block = es.enter_context(nc.Block())
        per = {e: [op for op in self.ops if op.eng == e] for e in self.ENGS}

        def run(eng_name, eobj):
            seen = {}
            for op in per[eng_name]:
                for d in op.deps:
                    if d.dma:
                        s, v = dsem[d.semkey], d.dmaval
                    else:
                        s, v = esem[d.eng], d.sigval
                    if seen.get(id(s), 0) < v:
                        eobj.wait_ge(s, v)
                        seen[id(s)] = v
                inst = op.fn(eobj)
                if op.dma:
                    inst.then_inc(dsem[op.semkey], 16)
                elif op.signalled:
                    inst.then_inc(esem[op.eng], 1)
            if eng_name == "sp":
                for k in final_dma_keys:
                    eobj.wait_ge(dsem[k], 16 * self.dma_count[k])

        @block.tensor
        def _(e):
            run("pe", e)

        @block.scalar
        def _(e):
            run("act", e)

        @block.vector
        def _(e):
            run("dve", e)

        @block.gpsimd
        def _(e):
            run("pool", e)

        @block.sync
        def _(e):
            run("sp", e)


def build(NSEQ, S):
    NTOK = NSEQ * S
    NTILE = S // T
    nc = bass.Bass("TRN2", target_bir_lowering=False)
    S_ = Sched()
    es = ExitStack()

    def dram(name, shape, dt, kind):
        return nc.dram_tensor(name, list(shape), dt, kind=kind).ap()

    x_d = dram("x", [NTOK, D], F32, "ExternalInput")
    wbig_d = dram("wbig", [NHU, 128, KC, 256], F32, "ExternalInput")
    wqkv_d = dram("wqkv", [3, 128, 8, 256], F32, "ExternalInput")
    wgate_d = dram("wgate", [2, 128, 8, 128], F32, "ExternalInput")
    wif_d = dram("wif", [128, 8, 8], F32, "ExternalInput")
    pvec_d = dram("pvec", [128, NPV, 8], F32, "ExternalInput")
    bif_d = dram("bif", [4, 2], F32, "ExternalInput")
    gpost_d = dram("gpost", [1, D], F32, "ExternalInput")
    cid_d = dram("c_ident", [128, 128], F32, "ExternalInput")
    cmask_d = dram("c_mask", [128, 128], F32, "ExternalInput")
    out_d = dram("out", [NTOK, D], F32, "ExternalOutput")
    wbf_d = dram("wbf", [NHU, 128, KC, 256], BF16, "Internal")

    def sb(name, shape, dt=F32):
        return es.enter_context(nc.sbuf_tensor("s_" + name, list(shape), dt))

    ring = sb("ring", [128, NRING, KC, 256], BF16)
    xt = sb("xt", [128, 2, D])
    xs = sb("xs", [128, D], BF16)
    xnT = sb("xnT", [128, KC, T], BF16)
    gTbc = sb("gTbc", [128, KC, 128])
    gpost_bc = sb("gpost_bc", [128, D])
    ident = sb("ident", [128, 128], BF16)
    ident32 = sb("ident32", [128, 128])
    maskT = sb("maskT", [128, 128])
    ones4 = sb("ones4", [4, 128])
    onesrow = sb("onesrow", [4, T])
    neghalf = sb("neghalf", [128, 4])
    eye4bc = sb("eye4bc", [4, 4, 4])
    pv = sb("pv", [128, NPV, 8])
    cv1 = sb("cv1", [128, 8])
    cvh = sb("cvh", [128, 8])
    hba = sb("hba", [128, 8])
    hbx = sb("hbx", [128, 8])
    ptmp = sb("ptmp", [128, 8])
    bif = sb("bif", [4, 2])
    nbf = sb("nbf", [4, 1])
    wq = sb("wq", [128, 8, 256], BF16)
    wk = sb("wk", [128, 8, 256], BF16)
    wv = sb("wv", [128, 8, 256], BF16)
    wa = sb("wa", [128, 8, 128], BF16)
    wx = sb("wx", [128, 8, 128], BF16)
    wif = sb("wif", [128, 8, 8], BF16)
    dgR = sb("dgR", [128, 2, 4, 128], BF16)
    dgM = sb("dgM", [128, 2, 4, 128], BF16)
    rxb = sb("rxb", [128, KC, 3 + T], BF16)
    mxb = sb("mxb", [128, KC, 3 + T], BF16)
    ss = sb("ss", [128, 4])
    ms = sb("ms", [128, 4])
    rstd = sb("rstd", [128, 4])
    ubf = sb("ubf", [128, 2, T], BF16)
    u32 = sb("u32", [128, 2, T])
    tr = sb("tr", [128, 2, T])
    ti = sb("ti", [128, 2, T])
    a_t = sb("a_t", [128, 2, T])
    a2s = sb("a2s", [128, 2, T])
    hh = sb("hh", [128, 2, T])
    tz = sb("tz", [128, 2, T])
    hst = sb("hst", [128, KC])
    yrT = sb("yrT", [128, KC, T], BF16)
    ymT = sb("ymT", [128, KC, T], BF16)
    Bc = sb("Bc", [4, 1])
    Gext = sb("Gext", [4, 5])
    dl0 = sb("dl0", [4, 4])
    dl = sb("dl", [4, 4])
    rhsd = sb("rhsd", [4, 4, 4])
    wtok = sb("wtok", [128, NSUB, 8])
    dlt = sb("dlt", [128, 16])
    cbf = sb("cbf", [128, 2, T], BF16)
    scm = sb("scm", [128, 2, T])
    qT = sb("qT", [128, 2, T], BF16)
    kT = sb("kT", [128, 2, T], BF16)
    kb = sb("kb", [128, NSUB, 256], BF16)
    wva = sb("wva", [128, NSUB, 260], BF16)
    t1sz = sb("t1sz", [128, 4, T])
    t1 = t1sz[:, 0:2, :]
    szm = t1sz[:, 2:4, :]
    c32 = t1
    to_ = sb("to_", [128, NSUB, 256])
    hmp = to_
    hn = sb("hn", [128, NSUB, 256], BF16)
    smT = sb("smT", [128, 2, 128], BF16)
    Cst = sb("Cst", [128, H + 1, 2, 260])
    Cbf = sb("Cbf", [128, 2, 2, 260], BF16)
    stats = sb("stats", [128, NSUB, 6])
    mv = sb("mv", [128, NSUB, 2])
    den = sb("den", [128, NSUB])
    epsv = sb("epsv", [128, NSUB])
    rs4 = sb("rs4", [128, NSUB])
    nmr = sb("nmr", [128, NSUB])
    trm = to_[:].rearrange("p (a b) c -> p a (b c)", a=2)
    y1 = t1sz
    y2 = sb("y2", [128, T])
    yT = sb("yT", [128, KC, T], BF16)
    obuf = sb("obuf", [128, D])
    ss2 = sb("ss2", [128, 2, 2])
    ms2 = sb("ms2", [128, 2])
    rs2 = sb("rs2", [128, 2])

    ps = es.enter_context(nc.psum_tensor("ps", [128, 8, 512], F32))

    def bank(b):
        return ps[:, b, :]

    def bank_bf(b):
        return ps[:, b, :].bitcast(BF16)

    class Rot:
        def __init__(self, banks):
            self.banks = list(banks)
            self.i = 0

        def __call__(self):
            b = self.banks[self.i % len(self.banks)]
            self.i += 1
            return b

    nb = Rot(range(8))
    nbR = Rot([0, 1])
    nbRz = Rot([2])
    nbSP = Rot([3, 4])
    nbDC = Rot([5])
    nbM = Rot([6, 7])

    def mm(out, lhsT, rhs, start, stop, reads, writes):
        S_.add("pe", lambda e: e.matmul(out, lhsT, rhs, start=start, stop=stop), reads, writes)

    def tp(out, in_, idn, reads, writes):
        S_.add("pe", lambda e: e.transpose(out, in_, idn), reads, writes)

    def act(out, in_, func, reads, writes, bias=None, scale=None, accum=None):
        kw = {}
        if bias is not None:
            kw["bias"] = bias
        if scale is not None:
            kw["scale"] = scale
        if accum is not None:
            kw["accum_out"] = accum
        S_.add("act", lambda e: e.activation(out, in_, func, **kw), reads, writes)

    def tt(eng, out, in0, in1, op, reads, writes):
        S_.add(eng, lambda e: e.tensor_tensor(out, in0, in1, op), reads, writes)

    def ts(eng, out, in0, s1, s2, op0, op1, reads, writes):
        if s2 is None:
            S_.add(eng, lambda e: e.tensor_scalar(out, in0, s1, None, op0), reads, writes)
        else:
            S_.add(eng, lambda e: e.tensor_scalar(out, in0, s1, s2, op0, op1), reads, writes)

    def stt(out, in0, scalar, in1, op0, op1, reads, writes):
        S_.add("dve", lambda e: e.scalar_tensor_tensor(out, in0, scalar, in1, op0, op1), reads, writes)

    def scan(out, d0, d1, init, op0, op1, reads, writes):
        S_.add("dve", lambda e: e.tensor_tensor_scan(out, d0, d1, init, op0, op1), reads, writes)

    def cp(eng, out, in_, reads, writes):
        S_.add(eng, lambda e: e.tensor_copy(out, in_), reads, writes)

    def dma(eng, out, in_, reads, writes, semkey, **kw):
        S_.add(eng, lambda e: e.dma_start(out=out, in_=in_, **kw), reads, writes, dma=True, semkey=semkey)

    def memset(eng, ap, val, writes):
        S_.add(eng, lambda e: e.memset(ap, val), (), writes)

    dma("sp", ident32[:], cid_d, (), ["ident32"], "ld_c1")
    dma("sp", maskT[:], cmask_d, (), ["maskT"], "ld_c2")
    dma("sp", pv[:], pvec_d, (), ["pv"], "ld_c3")
    dma("sp", bif[:], bif_d, (), ["bif"], "ld_c4")
    dma("sp", gpost_bc[:], gpost_d.partition_broadcast(128), (), ["gpost_bc"], "ld_c5")
    dma("pool", wif[:], wif_d, (), ["wif"], "ld_wif")
    memset("pool", ones4[:], 1.0, ["ones4"])
    memset("pool", onesrow[:], 1.0, ["onesrow"])
    memset("pool", neghalf[:], -0.5, ["neghalf"])
    memset("pool", wva[:], 0.0, ["wva"])
    memset("pool", Cbf[:], 0.0, ["Cbf"])
    memset("pool", rxb[:], 0.0, [("rxb", c) for c in range(KC)])
    memset("pool", mxb[:], 0.0, [("mxb", c) for c in range(KC)])
    cp("dve", ident[:], ident32[:], ["ident32"], ["ident"])
    for c in range(4):
        cp("dve", eye4bc[:, c, :], ident32[0:4, 0:4], ["ident32"], ["eye4bc"])
    for k in range(KC):
        cp("dve", gTbc[:, k, :], pv[:, PV_GPRE, k:k + 1].to_broadcast([128, 128]), ["pv"], ["gTbc"])
    act(ptmp[:], pv[:, PV_LAM, :], AF.Exp, ["pv"], ["ptmp"], scale=-1.0)
    act(ptmp[:], ptmp[:], AF.Ln, ["ptmp"], ["ptmp"], bias=1.0)
    ts("dve", cv1[:], ptmp[:], -8.0, None, ALU.mult, None, ["ptmp"], ["cv1"])
    ts("dve", cvh[:], ptmp[:], -4.0, None, ALU.mult, None, ["ptmp"], ["cvh"])
    ts("dve", hba[:], pv[:, PV_RBA, :], 0.5, None, ALU.mult, None, ["pv"], ["hba"])
    ts("dve", hbx[:], pv[:, PV_RBX, :], 0.5, None, ALU.mult, None, ["pv"], ["hbx"])
    ts("dve", nbf[:], bif[:, 1:2], -1.0, None, ALU.mult, None, ["bif"], ["nbf"])
    first_order = [HU_RX, HU_RZ, HU_MX, HU_MZ, HU_MO, HU_RX + 1, HU_RZ + 1, HU_MX + 1, HU_MZ + 1, HU_MO + 1]
    order = first_order + [g for g in range(NHU) if g not in first_order]
    small = [(wa, wgate_d[0], "wa"), (wx, wgate_d[1], "wx"), (wq, wqkv_d[0], "wq"), (wk, wqkv_d[1], "wk"),
             (wv, wqkv_d[2], "wv")]
    for i, g in enumerate(order):
        dma("pool", wbf_d[g], wbig_d[g], (), [("wbf", g)], ("wbf", g), max_dma_last_dim=4096)
        if i in (1, 3, 5, 7, 9):
            t_, d_, k_ = small[i // 2]
            dma("pool", t_[:], d_, (), [k_], "ld_" + k_, max_dma_last_dim=4096)

    ring_free = list(range(NRING))

    def load_hu(u):
        slot = ring_free.pop(0)
        dma("sp", ring[:, slot], wbf_d[u], [("wbf", u)], [("ring", slot)], ("ring", slot))
        return slot

    def release(slot):
        ring_free.append(slot)

    def lhs_hu(slot, k, j):
        return ring[:, slot, k, j * 128:(j + 1) * 128]

    def proj_fm(slot, j, b):
        for k in range(KC):
            mm(bank(b), lhs_hu(slot, k, j), xnT[:, k, :], k == 0, k == KC - 1,
               [("ring", slot), "xnT"], [("ps", b)])

    def build_diag(dg, par, row0, c, key):
        for jj in range(4):
            ts("pool", dg[:, par, jj, :], ident[:], pv[:, row0 + jj, c:c + 1], 0.0, ALU.mult, ALU.add,
               ["ident", "pv"], [(key, par)])

    def phase_X(tok0):
        for sub in range(NSUB):
            slot = sub % 2
            r0 = tok0 + sub * 128
            dma("sp", xt[:, slot, :], x_d[r0:r0 + 128, :], (), [("xt", slot)], ("xt", slot))
            act(xs[:], xt[:, slot, :], AF.Square, [("xt", slot)], ["xs", ("ss", sub)],
                accum=ss[:, sub:sub + 1])
            ts("dve", ms[:, sub:sub + 1], ss[:, sub:sub + 1], 1.0 / D, EPS, ALU.mult, ALU.add,
               [("ss", sub)], [("ms", sub)])
            tt("pool", rstd[:, sub:sub + 1], ms[:, sub:sub + 1], neghalf[:, 0:1], ALU.pow,
               [("ms", sub), "neghalf"], [("rstd", sub)])
            act(xs[:], xt[:, slot, :], AF.Copy, [("xt", slot), ("rstd", sub)], ["xs"],
                scale=rstd[:, sub:sub + 1])
            b = nb()
            for k in range(KC):
                tp(bank_bf(b)[:, k * 128:(k + 1) * 128], xs[:, k * 128:(k + 1) * 128], ident[:],
                   ["xs", "ident"], [("ps", b)])
            tt("dve", xnT[:, :, sub * 128:(sub + 1) * 128],
               bank_bf(b).rearrange("p (k t) -> p k t", k=KC), gTbc[:], ALU.mult,
               [("ps", b), "gTbc"], ["xnT"])

    def phase_G(first):
        bi_, bf_ = nb(), nb()
        for k in range(KC):
            mm(bank(bi_)[0:4, :], wif[:, k, 0:4], xnT[:, k, :], k == 0, k == KC - 1,
               ["wif", "xnT"], [("ps", bi_)])
        for k in range(KC):
            mm(bank(bf_)[0:4, :], wif[:, k, 4:8], xnT[:, k, :], k == 0, k == KC - 1,
               ["wif", "xnT"], [("ps", bf_)])
        if first:
            memset("dve", Bc[:], 0.0, ["Bc"])
            memset("dve", Gext[:], 0.0, ["Gext"])
            memset("dve", hst[:], 0.0, ["hst"])
            memset("dve", Cst[:], 0.0, [("Cst", h_) for h_ in range(H)] + ["Ctmp"])
            memset("pool", rxb[:, :, 0:3], 0.0, [("rxb", c) for c in range(KC)])
            memset("pool", mxb[:, :, 0:3], 0.0, [("mxb", c) for c in range(KC)])
        else:
            cp("dve", Gext[:, 0:1], Gext[:, 4:5], ["Gext"], ["Gext"])
        lfv, Btv, Atv, Gtv, E1v, E2v = (u32[0:4, 0, :], tr[0:4, 0, :], ti[0:4, 0, :], a_t[0:4, 0, :],
                                         a2s[0:4, 0, :], tz[0:4, 0, :])
        kl, kB, kA, kG, k1, k2 = ("u32", 0), ("tr", 0), ("ti", 0), ("a_t", 0), ("a2s", 0), ("tz", 0)
        c3 = "p (c t) -> p c t"
        act(lfv, bank(bf_)[0:4, :], AF.Exp, [("ps", bf_), "nbf"], [kl], bias=nbf[:, 0:1], scale=-1.0)
        act(lfv, lfv, AF.Ln, [kl], [kl], bias=1.0)
        scan(Btv, onesrow[:], lfv, Bc[:, 0:1], ALU.mult, ALU.subtract, ["onesrow", kl, "Bc"], [kB])
        cp("dve", Bc[:], Btv[:, T - 1:T], [kB], ["Bc"])
        stt(Atv, bank(bi_)[0:4, :], bif[:, 0:1], Btv, ALU.add, ALU.subtract, [("ps", bi_), "bif", kB], [kA])
        scan(Gtv, Atv, Atv, Gext[:, 0:1], ALU.max, ALU.max, [kA, "Gext"], [kG])
        Gv = Gtv.rearrange(c3, t=128)
        cp("dve", Gext[:, 1:5].unsqueeze(2), Gv[:, :, 127:128], [kG], ["Gext"])
        tt("dve", dl0[:], Gext[:, 0:4], Gext[:, 1:5], ALU.subtract, ["Gext"], ["dl0"])
        act(dl[:], dl0[:], AF.Exp, ["dl0"], ["dl"])
        Rb = Gext[:, 1:5].unsqueeze(2).to_broadcast([4, 4, 128])
        tt("dve", E1v.rearrange(c3, t=128), Atv.rearrange(c3, t=128), Rb, ALU.subtract, [kA, "Gext"], [k1])
        act(E1v, E1v, AF.Exp, [k1], [k1])
        tt("dve", E2v.rearrange(c3, t=128), Btv.rearrange(c3, t=128), Rb, ALU.add, [kB, "Gext"], [k2])
        act(E2v, E2v, AF.Exp, [k2], [k2], scale=-1.0)
        bt_ = nb()
        for c in range(NSUB):
            mm(bank(bt_)[:, c * 8:c * 8 + 4], E1v[:, c * 128:(c + 1) * 128], ident32[0:4, 0:4], True, True,
               [k1, "ident32"], [("ps", bt_)])
            mm(bank(bt_)[:, c * 8 + 4:c * 8 + 8], E2v[:, c * 128:(c + 1) * 128], ident32[0:4, 0:4], True, True,
               [k2, "ident32"], [("ps", bt_)])
        cp("dve", wtok[:].rearrange("p c j -> p (c j)"), bank(bt_)[:, 0:32], [("ps", bt_)], ["wtok"])
        tt("dve", rhsd[:], dl[:].unsqueeze(2).to_broadcast([4, 4, 4]), eye4bc[:], ALU.mult,
           ["dl", "eye4bc"], ["rhsd"])
        bd_ = nb()
        mm(bank(bd_)[:, 0:16], ones4[:], rhsd[:].rearrange("p c h -> p (c h)"), True, True,
           ["ones4", "rhsd"], [("ps", bd_)])
        cp("dve", dlt[:], bank(bd_)[:, 0:16], [("ps", bd_)], ["dlt"])

    def gen_R(pre):
        s_rx, s_rz = pre
        for c in range(KC):
            p = c % 2
            j = c % 2
            if c > 0 and j == 0:
                s_rx = load_hu(HU_RX + c // 2)
                s_rz = load_hu(HU_RZ + c // 2)
            build_diag(dgR, p, PV_RCW, c, "dgR")
            b_rx = nbR()
            proj_fm(s_rx, j, b_rx)
            cp("dve", rxb[:, c, 3:3 + T], bank(b_rx), [("ps", b_rx)], [("rxb", c)])
            yield
            b_u = nbR()
            for jj in range(4):
                mm(bank(b_u), dgR[:, p, jj, :], rxb[:, c, jj:jj + T], jj == 0, jj == 3,
                   [("dgR", p), ("rxb", c)], [("ps", b_u)])
            act(ubf[:, p, :], bank(b_u), AF.Identity, [("ps", b_u), "pv"], [("ubf", p)],
                bias=pv[:, PV_RCB, c:c + 1])
            act(u32[:, p, :], bank(b_u), AF.Identity, [("ps", b_u), "pv"], [("u32", p)],
                bias=pv[:, PV_RCB, c:c + 1])
            cp("pool", rxb[:, c, 0:3], rxb[:, c, T:T + 3], [("rxb", c)], [("rxb", c)])
            yield
            b_a, b_x = nbR(), nbR()
            mm(bank(b_a), wa[:, c, :], ubf[:, p, :], True, True, ["wa", ("ubf", p)], [("ps", b_a)])
            mm(bank(b_x), wx[:, c, :], ubf[:, p, :], True, True, ["wx", ("ubf", p)], [("ps", b_x)])
            act(tr[:, p, :], bank(b_a), AF.Tanh, [("ps", b_a), "hba"], [("tr", p)], bias=hba[:, c:c + 1], scale=0.5)
            act(ti[:, p, :], bank(b_x), AF.Tanh, [("ps", b_x), "hbx"], [("ti", p)], bias=hbx[:, c:c + 1], scale=0.5)
            act(a_t[:, p, :], tr[:, p, :], AF.Exp, [("tr", p), "cvh"], [("a_t", p)],
                bias=cvh[:, c:c + 1], scale=cvh[:, c:c + 1])
            act(a2s[:, p, :], tr[:, p, :], AF.Exp, [("tr", p), "cv1"], [("a2s", p)],
                bias=cv1[:, c:c + 1], scale=cv1[:, c:c + 1])
            act(a2s[:, p, :], a2s[:, p, :], AF.Sqrt, [("a2s", p)], [("a2s", p)], bias=1.0, scale=-1.0)
            stt(u32[:, p, :], ti[:, p, :], 1.0, u32[:, p, :], ALU.add, ALU.mult, [("ti", p), ("u32", p)], [("u32", p)])
            tt("dve", a2s[:, p, :], a2s[:, p, :], u32[:, p, :], ALU.mult, [("a2s", p), ("u32", p)], [("a2s", p)])
            scan(hh[:, p, :], a_t[:, p, :], a2s[:, p, :], hst[:, c:c + 1], ALU.mult, ALU.add,
                 [("a_t", p), ("a2s", p), "hst"], [("hh", p)])
            cp("dve", hst[:, c:c + 1], hh[:, p, T - 1:T], [("hh", p)], ["hst"])
            yield
            b_rz = nbRz()
            proj_fm(s_rz, j, b_rz)
            act(tz[:, p, :], bank(b_rz), AF.Tanh, [("ps", b_rz)], [("tz", p)], scale=0.5)
            stt(hh[:, p, :], bank(b_rz), 0.25, hh[:, p, :], ALU.mult, ALU.mult, [("ps", b_rz), ("hh", p)], [("hh", p)])
            stt(yrT[:, c, :], tz[:, p, :], 1.0, hh[:, p, :], ALU.add, ALU.mult, [("tz", p), ("hh", p)], [("yrT", c)])
            if j == 1:
                release(s_rx)
                release(s_rz)
            yield

    def gen_M(pre):
        s_mx, s_mz, s_mo = pre
        for h_ in range(H):
            if h_ > 0:
                s_mx, s_mz, s_mo = nxt_slots
            for e2 in range(2):
                c = 2 * h_ + e2
                build_diag(dgM, e2, PV_MCW, c, "dgM")
                b_mx = nbM()
                proj_fm(s_mx, e2, b_mx)
                cp("dve", mxb[:, c, 3:3 + T], bank(b_mx), [("ps", b_mx)], [("mxb", c)])
            release(s_mx)
            yield "dense"
            for e2 in range(2):
                c = 2 * h_ + e2
                b_c = nbM()
                for jj in range(4):
                    mm(bank(b_c), dgM[:, e2, jj, :], mxb[:, c, jj:jj + T], jj == 0, jj == 3,
                       [("dgM", e2), ("mxb", c)], [("ps", b_c)])
                act(c32[:, e2, :], bank(b_c), AF.Silu, [("ps", b_c), "pv"], [("t1", e2)],
                    bias=pv[:, PV_MCB, c:c + 1])
                act(cbf[:, e2, :], c32[:, e2, :], AF.Copy, [("t1", e2)], [("cbf", e2)])
                act(scm[:, e2, :], c32[:, e2, :], AF.Copy, [("t1", e2), "pv"], [("scm", e2)],
                    scale=pv[:, PV_MSK, c:c + 1])
                b_mz = nbM()
                proj_fm(s_mz, e2, b_mz)
                act(szm[:, e2, :], bank(b_mz), AF.Silu, [("ps", b_mz)], [("szm", e2)])
            release(s_mz)
            yield "dense"
            for e2 in range(2):
                b_q = nbM()
                for kc in range(2):
                    mm(bank(b_q), wq[:, h_ * 2 + kc, e2 * 128:(e2 + 1) * 128], cbf[:, kc, :], kc == 0, kc == 1,
                       ["wq", ("cbf", 0), ("cbf", 1)], [("ps", b_q)])
                act(qT[:, e2, :], bank(b_q), AF.Copy, [("ps", b_q)], [("qT", e2)])
                b_k = nbM()
                for kc in range(2):
                    mm(bank(b_k), wk[:, h_ * 2 + kc, e2 * 128:(e2 + 1) * 128], cbf[:, kc, :], kc == 0, kc == 1,
                       ["wk", ("cbf", 0), ("cbf", 1)], [("ps", b_k)])
                act(kT[:, e2, :], bank(b_k), AF.Copy, [("ps", b_k)], [("kT", e2)], scale=1.0 / 16.0)
            yield "dense"
            cp("dve", wva[:, :, 256], wtok[:, :, h_], ["wtok"], ["wva"])
            for sub in range(NSUB):
                tsl = slice(sub * 128, (sub + 1) * 128)
                b_kv = nbM()
                for kc in range(2):
                    mm(bank(b_kv)[:, 0:256], cbf[:, kc, tsl], wk[:, h_ * 2 + kc, :], kc == 0, kc == 1,
                       ["wk", ("cbf", 0), ("cbf", 1)], [("ps", b_kv)])
                for kc in range(2):
                    mm(bank(b_kv)[:, 256:512], mxb[:, 2 * h_ + kc, 3 + sub * 128:3 + (sub + 1) * 128],
                       wv[:, h_ * 2 + kc, :], kc == 0, kc == 1,
                       ["wv", ("mxb", 2 * h_), ("mxb", 2 * h_ + 1)], [("ps", b_kv)])
                act(kb[:, sub, :], bank(b_kv)[:, 0:256], AF.Copy, [("ps", b_kv)], ["kb"], scale=1.0 / 16.0)
                act(wva[:, sub, 0:256], bank(b_kv)[:, 256:512], AF.Copy, [("ps", b_kv), "wtok"], ["wva"],
                    scale=wtok[:, sub, h_:h_ + 1])
                b_o = nbM()
                for k in range(KC):
                    mm(bank(b_o)[:, 0:256], xnT[:, k, tsl], ring[:, s_mo, k, :],
                       k == 0, k == KC - 1, ["xnT", ("ring", s_mo)], [("ps", b_o)])
                act(to_[:, sub, :], bank(b_o)[:, 0:256], AF.Tanh, [("ps", b_o)], [("to_", sub)], scale=0.5)
                yield "dense"
            release(s_mo)
            for e2 in range(2):
                c = 2 * h_ + e2
                cp("pool", mxb[:, c, 0:3], mxb[:, c, T:T + 3], [("mxb", c)], [("mxb", c)])
            if h_ + 1 < H:
                nxt_slots = (load_hu(HU_MX + h_ + 1), load_hu(HU_MZ + h_ + 1), load_hu(HU_MO + h_ + 1))
            for sub in range(NSUB):
                tsl = slice(sub * 128, (sub + 1) * 128)
                par = sub % 2
                hc = h_ if par == 0 else H
                hn_ = H if par == 0 else h_
                dsc = dlt[:, sub * 4 + h_:sub * 4 + h_ + 1]
                skeys = [("Cst", h_), "Ctmp"]
                act(Cbf[:, par, :, 0:257], Cst[:, hc, :, 0:257], AF.Copy, skeys + ["dlt"], [("Cbf", par)], scale=dsc)
                b_sp = nbSP()
                for kc in range(2):
                    mm(bank(b_sp)[:, 0:128], kT[:, kc, tsl], qT[:, kc, tsl], kc == 0, kc == 1,
                       [("kT", 0), ("kT", 1), ("qT", 0), ("qT", 1)], [("ps", b_sp)])
                tt("dve", smT[:, par, :], bank(b_sp)[:, 0:128], maskT[:], ALU.mult,
                   [("ps", b_sp), "maskT"], [("smT", par)])
                b_dc = nbDC()
                for kc in range(2):
                    mm(bank(b_dc)[:, kc * 256:(kc + 1) * 256], kb[:, sub, kc * 128:(kc + 1) * 128],
                       wva[:, sub, 0:256], True, True, ["kb", "wva"], [("ps", b_dc)])
                yield "lat"
                Pv = bank(b_sp)[:, 128:385]
                mm(Pv, smT[:, par, :], wva[:, sub, 0:257], True, False, [("smT", par), "wva"], [("ps", b_sp)])
                for kc in range(2):
                    mm(Pv, qT[:, kc, tsl], Cbf[:, par, kc, 0:257], False, kc == 1,
                       [("qT", 0), ("qT", 1), ("Cbf", par)], [("ps", b_sp)])
                for kc in range(2):
                    mm(bank(b_sp)[:, 400 + 2 * kc:402 + 2 * kc], kb[:, sub, kc * 128:(kc + 1) * 128],
                       wva[:, sub, 256:258], True, True, ["kb", "wva"], [("ps", b_sp)])
                stt(Cst[:, hn_, :, 0:256], Cst[:, hc, :, 0:256], dsc,
                    bank(b_dc).rearrange("p (k e) -> p k e", k=2), ALU.mult, ALU.add,
                    skeys + ["dlt", ("ps", b_dc)], skeys)
                stt(Cst[:, hn_, :, 256], Cst[:, hc, :, 256], dsc,
                    bank(b_sp)[:, 400:404].rearrange("p (k t) -> p k t", t=2)[:, :, 0], ALU.mult, ALU.add,
                    skeys + ["dlt", ("ps", b_sp)], skeys)
                stt(hmp[:, sub, :], to_[:, sub, :], 1.0, Pv[:, 0:256], ALU.add, ALU.mult,
                    [("to_", sub), ("ps", b_sp)], [("to_", sub)])
                ts("dve", den[:, sub:sub + 1], Pv[:, 256:257], -1.0, wtok[:, sub, 4 + h_:5 + h_],
                   ALU.mult, ALU.max, [("ps", b_sp), "wtok"], [("den", sub)])
                tt("dve", den[:, sub:sub + 1], den[:, sub:sub + 1], Pv[:, 256:257], ALU.max,
                   [("ps", b_sp), ("den", sub)], [("den", sub)])
                S_.add("dve", (lambda o, i: (lambda e: e.bn_stats(o, i)))(stats[:, sub, :], hmp[:, sub, :]),
                       [("to_", sub)], [("stats", sub)])
                S_.add("dve", (lambda o, i: (lambda e: e.bn_aggr(o, i)))(mv[:, sub, :], stats[:, sub, :]),
                       [("stats", sub)], [("mv", sub)])
                yield "lat"
            allsub = list(range(NSUB))
            tt("dve", epsv[:], den[:], den[:], ALU.mult, [("den", s) for s in allsub], ["epsv"])
            stt(epsv[:], epsv[:], 4.0 * EPS, mv[:, :, 1], ALU.mult, ALU.add,
                ["epsv"] + [("mv", s) for s in allsub], ["epsv"])
            tt("pool", rs4[:], epsv[:], neghalf[:], ALU.pow, ["epsv", "neghalf"], ["rs4"])
            stt(nmr[:], mv[:, :, 0], -1.0, rs4[:], ALU.mult, ALU.mult, [("mv", s) for s in allsub] + ["rs4"], ["nmr"])
            yield "lat"
            b_t = nbM()
            for sub in range(NSUB):
                act(hn[:, sub, :], hmp[:, sub, :], AF.Identity, [("to_", sub), "rs4", "nmr"], [("hn", sub)],
                    bias=nmr[:, sub:sub + 1], scale=rs4[:, sub:sub + 1])
                for e2 in range(2):
                    tp(bank_bf(b_t)[:, e2 * 512 + sub * 128:e2 * 512 + (sub + 1) * 128],
                       hn[:, sub, e2 * 128:(e2 + 1) * 128], ident[:], [("hn", sub), "ident"], [("ps", b_t)])
            yield "lat"
            for e2 in range(2):
                c = 2 * h_ + e2
                stt(t1[:, e2, :], bank_bf(b_t)[:, e2 * 512:(e2 + 1) * 512], pv[:, PV_MNG, c:c + 1], scm[:, e2, :],
                    ALU.mult, ALU.add, [("ps", b_t), "pv", ("scm", e2)], [("t1", e2)])
                tt("dve", ymT[:, c, :], t1[:, e2, :], szm[:, e2, :], ALU.mult, [("t1", e2), ("szm", e2)], [("ymT", c)])
            yield "lat"

    def phase_F(tile, tok0):
        for q in range(4):
            s_gr = load_hu(HU_GR + q)
            s_br = load_hu(HU_BR + q)
            s_gm = load_hu(HU_GM + q)
            s_bm = load_hu(HU_BM + q)
            for j in range(2):
                b_g = nb()
                proj_fm(s_gr, j, b_g)
                act(trm[:, j, :], bank(b_g), AF.Tanh, [("ps", b_g)], [("to_", 2 * j), ("to_", 2 * j + 1)], scale=0.5)
                b_r = nb()
                for k in range(KC):
                    mm(bank(b_r), lhs_hu(s_br, k, j), yrT[:, k, :], k == 0, k == KC - 1,
                       [("ring", s_br), ("yrT", k)], [("ps", b_r)])
                stt(y1[:, j, :], trm[:, j, :], 1.0, bank(b_r), ALU.add, ALU.mult,
                    [("to_", 2 * j), ("to_", 2 * j + 1), ("ps", b_r)], [("t1", j)])
            release(s_gr)
            release(s_br)
            for j in range(2):
                dc = q * 2 + j
                b_g = nb()
                proj_fm(s_gm, j, b_g)
                act(trm[:, j, :], bank(b_g), AF.Tanh, [("ps", b_g)], [("to_", 2 * j), ("to_", 2 * j + 1)], scale=0.5)
                b_m = nb()
                for k in range(KC):
                    mm(bank(b_m), lhs_hu(s_bm, k, j), ymT[:, k, :], k == 0, k == KC - 1,
                       [("ring", s_bm), ("ymT", k)], [("ps", b_m)])
                stt(y2[:], trm[:, j, :], 1.0, bank(b_m), ALU.add, ALU.mult,
                    [("to_", 2 * j), ("to_", 2 * j + 1), ("ps", b_m)], ["y2"])
                tt("dve", yT[:, dc, :], y1[:, j, :], y2[:], ALU.add, [("t1", j), "y2"], [("yT", dc)])
            release(s_gm)
            release(s_bm)
        s_o = [load_hu(HU_WO + q) for q in range(4)]
        for sub in range(NSUB):
            tsl = slice(sub * 128, (sub + 1) * 128)
            osl = sub % 2
            r0 = tok0 + sub * 128
            bo = [nb(), nb()]
            for hf in range(2):
                for q2 in range(2):
                    q = hf * 2 + q2
                    for k in range(KC):
                        mm(bank(bo[hf])[:, q2 * 256:(q2 + 1) * 256], yT[:, k, tsl], ring[:, s_o[q], k, :],
                           k == 0, k == KC - 1, [("yT", k), ("ring", s_o[q])], [("ps", bo[hf])])
                act(xs[:, 0:512], bank(bo[hf]), AF.Square, [("ps", bo[hf])], ["xs", ("ss2", osl, hf)],
                    accum=ss2[:, osl, hf:hf + 1])
            tt("dve", ms2[:, osl:osl + 1], ss2[:, osl, 0:1], ss2[:, osl, 1:2], ALU.add,
               [("ss2", osl, 0), ("ss2", osl, 1)], [("ms2", osl)])
            ts("dve", ms2[:, osl:osl + 1], ms2[:, osl:osl + 1], 1.0 / D, 4.0 * EPS, ALU.mult, ALU.add,
               [("ms2", osl)], [("ms2", osl)])
            tt("pool", rs2[:, osl:osl + 1], ms2[:, osl:osl + 1], neghalf[:, 0:1], ALU.pow,
               [("ms2", osl), "neghalf"], [("rs2", osl)])
            dma("sp", xt[:, osl, :], x_d[r0:r0 + 128, :], (), [("xt", osl)], ("xt", osl))
            for hf in range(2):
                stt(obuf[:, hf * 512:(hf + 1) * 512], bank(bo[hf]), rs2[:, osl:osl + 1],
                    gpost_bc[:, hf * 512:(hf + 1) * 512], ALU.mult, ALU.mult,
                    [("ps", bo[hf]), ("rs2", osl), "gpost_bc"], ["obuf"])
            tt("pool", obuf[:], obuf[:], xt[:, osl, :], ALU.add, ["obuf", ("xt", osl)], ["obuf"])
            dma("sp", out_d[r0:r0 + 128, :], obuf[:], ["obuf"], [("out", tile, sub)], ("st", 0))
        for q in range(4):
            release(s_o[q])

    for tile in range(NSEQ * NTILE):
        seq, tl = divmod(tile, NTILE)
        first = tl == 0
        tok0 = seq * S + tl * T
        preR = (load_hu(HU_RX), load_hu(HU_RZ))
        preM = (load_hu(HU_MX), load_hu(HU_MZ), load_hu(HU_MO))
        import os as _os
        DBG = _os.environ.get("KDBG", "")
        phase_X(tok0)
        if DBG == "X":
            continue
        phase_G(first)
        if DBG == "G":
            continue
        gR = gen_R(preR)
        gM = gen_M(preM)
        if DBG == "R":
            for _ in gR:
                pass
            continue
        if DBG == "M":
            for _ in gM:
                pass
            continue
        if DBG == "SEQ":
            for _ in gR:
                pass
            for _ in gM:
                pass
            continue
        if DBG == "SEQ2":
            for _ in gM:
                pass
            for _ in gR:
                pass
            continue
        r_alive = True
        _stride = int(_os.environ.get("KSTRIDE", "1"))
        _burst = int(_os.environ.get("KBURST", "1"))
        _n = 0
        for tag in gM:
            if tag == (_os.environ.get("KTAG", "lat")) and r_alive:
                _n += 1
                if _n % _stride:
                    continue
                try:
                    for _b in range(_burst):
                        next(gR)
                except StopIteration:
                    r_alive = False
        if r_alive:
            for _ in gR:
                pass
        if DBG == "RM":
            continue
        phase_F(tile, tok0)
        assert len(ring_free) == NRING, ring_free
    S_.emit(nc, es, [k for k in [("st", 0)] if k in S_.dma_count])
    es.close()
    return nc


def prep_weights(inp):
    f = np.float32
    w_in = np.asarray(inp["w_in"][0], f)
    cols = {}
    offs = [0, 1024, 2048, 3072, 4096, 5120, 5124, 5128, 6152, 7176]
    names = ["r_x", "r_z", "m_x", "m_z", "m_o", "m_i", "m_f", "gate_r", "gate_m"]
    for n, a, b in zip(names, offs[:-1], offs[1:]):
        cols[n] = w_in[:, a:b]
    big = [cols["r_x"], cols["r_z"], cols["m_x"], cols["m_z"], cols["m_o"], cols["gate_r"], cols["gate_m"],
           np.asarray(inp["w_branch_r"][0], f), np.asarray(inp["w_branch_m"][0], f), np.asarray(inp["w_out"][0], f)]
    units = []
    for W in big:
        for q in range(4):
            Wh = W[:, q * 256:(q + 1) * 256]
            units.append(Wh.reshape(8, 128, 256).transpose(1, 0, 2))
    wbig = np.ascontiguousarray(np.stack(units, 0))
    qkv = []
    for n in ("ml_w_q", "ml_w_k", "ml_w_v"):
        W = np.asarray(inp[n][0], f)
        qkv.append(W.reshape(4, 2, 128, 256).transpose(2, 0, 1, 3).reshape(128, 8, 256))
    wqkv = np.ascontiguousarray(np.stack(qkv, 0))
    gates = []
    for n in ("rg_w_a", "rg_w_x"):
        W = np.asarray(inp[n][0], f)
        G = np.zeros((128, 8, 128), f)
        for c in range(8):
            G[0:64, c, 0:64] = W[2 * c]
            G[64:128, c, 64:128] = W[2 * c + 1]
        gates.append(G)
    wgate = np.ascontiguousarray(np.stack(gates, 0))
    wif = np.ascontiguousarray(w_in[:, 5120:5128].reshape(8, 128, 8).transpose(1, 0, 2))
    pnames = ["g_pre", "rg_conv_b", "rg_b_a", "rg_b_x", "rg_lambda", "ml_conv_b", "ml_norm_g", "ml_skip"]
    prow = [np.asarray(inp[n][0], f) for n in pnames]
    prow += [np.asarray(inp["rg_conv_w"][0], f)[j] for j in range(4)]
    prow += [np.asarray(inp["ml_conv_w"][0], f)[j] for j in range(4)]
    pvec = np.ascontiguousarray(np.stack([r.reshape(8, 128).T for r in prow], 1))
    bif = np.ascontiguousarray(np.stack([np.asarray(inp["ml_b_i"][0], f), np.asarray(inp["ml_b_f"][0], f)], 1))
    gpost = np.ascontiguousarray(np.asarray(inp["g_post"][0], f).reshape(1, 1024))
    c_ident = np.eye(128, dtype=f)
    c_mask = np.triu(np.ones((128, 128), f))
    return dict(wbig=wbig, wqkv=wqkv, wgate=wgate, wif=wif, pvec=pvec, bif=bif, gpost=gpost,
                c_ident=c_ident, c_mask=c_mask)


_NC_CACHE = {}


def kernel(**inputs):
    x = np.asarray(inputs["x"], np.float32)
    B, S, _ = x.shape
    ncores = 8
    nseq = B // ncores
    key = (nseq, S)
    if key not in _NC_CACHE:
        _NC_CACHE[key] = build(nseq, S)
    nc = _NC_CACHE[key]
    wts = prep_weights(inputs)
    in_maps = []
    for c in range(ncores):
        m = dict(wts)
        m["x"] = np.ascontiguousarray(x[c * nseq:(c + 1) * nseq].reshape(nseq * S, D))
        in_maps.append(m)
    res = run_bass_kernel_spmd(nc, in_maps, core_ids=list(range(ncores)))
    outs = [np.asarray(r["out"], np.float32).reshape(nseq, S, D) for r in res.results]
    return np.concatenate(outs, 0)
```

```python
import numpy as np
from contextlib import ExitStack
import concourse.bass as bass
import concourse.mybir as mybir
from concourse.bass_utils import run_bass_kernel_spmd

F32 = mybir.dt.float32
BF16 = mybir.dt.bfloat16
ALU = mybir.AluOpType
AF = mybir.ActivationFunctionType

D = 1024
KC = 8
T = 512
NSUB = 4
H = 4
EPS = 1e-6
NHU = 40
NRING = 9
HU_RX, HU_RZ, HU_MX, HU_MZ, HU_MO, HU_GR, HU_GM, HU_BR, HU_BM, HU_WO = 0, 4, 8, 12, 16, 20, 24, 28, 32, 36
PV_GPRE, PV_RCB, PV_RBA, PV_RBX, PV_LAM, PV_MCB, PV_MNG, PV_MSK = range(8)
PV_RCW = 8
PV_MCW = 12
NPV = 16


class _Op:
    __slots__ = ("eng", "fn", "deps", "dma", "semkey", "sigval", "signalled", "dmaval", "multi")

    def __init__(self, eng, fn, dma, semkey):
        self.multi = False
        self.eng = eng
        self.fn = fn
        self.deps = []
        self.dma = dma
        self.semkey = semkey
        self.sigval = 0
        self.signalled = False
        self.dmaval = 0


class Sched:
    ENGS = ("pe", "act", "dve", "pool", "sp")

    def __init__(self):
        self.ops = []
        self.last_writer = {}
        self.readers = {}
        self.dma_count = {}

    def add(self, eng, fn, reads=(), writes=(), dma=False, semkey=None, multi=False):
        op = _Op(eng, fn, dma, semkey)
        op.multi = multi
        deps = {}
        for r in reads:
            w = self.last_writer.get(r)
            if w is not None:
                deps[id(w)] = (w, True)
            if isinstance(r, tuple) and r[0] == "ps":
                for rd in self.readers.get(r, ()):
                    if rd.eng != eng and id(rd) not in deps:
                        deps[id(rd)] = (rd, False)
        for k in writes:
            w = self.last_writer.get(k)
            if w is not None and id(w) not in deps:
                deps[id(w)] = (w, False)
            for rd in self.readers.get(k, ()):
                if id(rd) not in deps:
                    deps[id(rd)] = (rd, False)
        for d, raw in deps.values():
            if d is op:
                continue
            if d.dma:
                op.deps.append(d)
            elif d.eng != eng:
                op.deps.append(d)
            elif eng != "pe" and not dma:
                op.deps.append(d)
            elif dma and not d.dma:
                op.deps.append(d)
        for k in writes:
            self.last_writer[k] = op
            self.readers[k] = []
        for r in reads:
            if r not in writes:
                self.readers.setdefault(r, []).append(op)
        if dma:
            n = self.dma_count.get(semkey, 0) + 1
            self.dma_count[semkey] = n
            op.dmaval = 16 * n
        self.ops.append(op)
        return op

    def finalize(self):
        for op in self.ops:
            for d in op.deps:
                if not d.dma:
                    d.signalled = True
        cnt = {e: 0 for e in self.ENGS}
        for op in self.ops:
            if op.signalled and not op.dma:
                cnt[op.eng] += 1
                op.sigval = cnt[op.eng]

    def emit(self, nc, es, final_dma_keys):
        self.finalize()
        esem = {e: es.enter_context(nc.semaphore("sem_" + e)) for e in self.ENGS}
        dsem = {}
        for k in self.dma_count:
            dsem[k] = es.enter_context(nc.semaphore("dsem_%d" % len(dsem)))
        block = es.enter_context(nc.Block())
        per = {e: [op for op in self.ops if op.eng == e] for e in self.ENGS}

        def run(eng_name, eobj):
            seen = {}
            for op in per[eng_name]:
                need = {}
                for d in op.deps:
                    if d.dma:
                        s, v = dsem[d.semkey], d.dmaval
                    else:
                        s, v = esem[d.eng], d.sigval
                    if seen.get(id(s), 0) < v and need.get(id(s), (None, 0))[1] < v:
                        need[id(s)] = (s, v)
                need = list(need.values())
                embed = None
                if need and eng_name in ("act", "dve", "pool") and not op.dma and not op.multi:
                    embed = need.pop()
                for s, v in need:
                    eobj.wait_ge(s, v)
                    seen[id(s)] = v
                inst = op.fn(eobj)
                if embed is not None:
                    inst._wait_ge(embed[0], embed[1])
                    seen[id(embed[0])] = embed[1]
                if op.dma:
                    inst.then_inc(dsem[op.semkey], 16)
                elif op.signalled:
                    inst.then_inc(esem[op.eng], 1)
            if eng_name == "sp":
                for k in final_dma_keys:
                    eobj.wait_ge(dsem[k], 16 * self.dma_count[k])

        @block.tensor
        def _(e):
            run("pe", e)

        @block.scalar
        def _(e):
            run("act", e)

        @block.vector
        def _(e):
            run("dve", e)

        @block.gpsimd
        def _(e):
            run("pool", e)

        @block.sync
        def _(e):
            run("sp", e)


def build(NSEQ, S):
    NTOK = NSEQ * S
    NTILE = S // T
    nc = bass.Bass("TRN2", target_bir_lowering=False)
    S_ = Sched()
    es = ExitStack()

    def dram(name, shape, dt, kind):
        return nc.dram_tensor(name, list(shape), dt, kind=kind).ap()

    x_d = dram("x", [NTOK, D], F32, "ExternalInput")
    wbig_d = dram("wbig", [NHU, 128, KC, 256], F32, "ExternalInput")
    wqkv_d = dram("wqkv", [3, 128, 8, 256], F32, "ExternalInput")
    wgate_d = dram("wgate", [2, 128, 8, 128], F32, "ExternalInput")
    wif_d = dram("wif", [128, 8, 8], F32, "ExternalInput")
    pvec_d = dram("pvec", [128, NPV, 8], F32, "ExternalInput")
    bif_d = dram("bif", [4, 2], F32, "ExternalInput")
    gpost_d = dram("gpost", [1, D], F32, "ExternalInput")
    cid_d = dram("c_ident", [128, 128], F32, "ExternalInput")
    cmask_d = dram("c_mask", [128, 128], F32, "ExternalInput")
    out_d = dram("out", [NTOK, D], F32, "ExternalOutput")
    wbf_d = dram("wbf", [NHU, 128, KC, 256], BF16, "Internal")

    def sb(name, shape, dt=F32):
        return es.enter_context(nc.sbuf_tensor("s_" + name, list(shape), dt))

    ring = sb("ring", [128, NRING, KC, 256], BF16)
    xt = sb("xt", [128, 2, D])
    xs = sb("xs", [128, D], BF16)
    xnT = sb("xnT", [128, KC, T], BF16)
    gTbc = sb("gTbc", [128, KC, 128])
    gpost_bc = sb("gpost_bc", [128, D])
    ident = sb("ident", [128, 128], BF16)
    ident32 = sb("ident32", [128, 128])
    maskT = sb("maskT", [128, 128])
    ones4 = sb("ones4", [4, 128])
    onesrow = sb("onesrow", [4, T])
    neghalf = sb("neghalf", [128, 4])
    eye4bc = sb("eye4bc", [4, 4, 4])
    pv = sb("pv", [128, NPV, 8])
    cv1 = sb("cv1", [128, 8])
    cvh = sb("cvh", [128, 8])
    hba = sb("hba", [128, 8])
    hbx = sb("hbx", [128, 8])
    ptmp = sb("ptmp", [128, 8])
    bif = sb("bif", [4, 2])
    nbf = sb("nbf", [4, 1])
    wq = sb("wq", [128, 8, 256], BF16)
    wk = sb("wk", [128, 8, 256], BF16)
    wv = sb("wv", [128, 8, 256], BF16)
    wa = sb("wa", [128, 8, 128], BF16)
    wx = sb("wx", [128, 8, 128], BF16)
    wif = sb("wif", [128, 8, 8], BF16)
    dgR = sb("dgR", [128, 2, 4, 128], BF16)
    dgM = sb("dgM", [128, 2, 4, 128], BF16)
    rxb = sb("rxb", [128, KC, 3 + T], BF16)
    mxb = sb("mxb", [128, KC, 3 + T], BF16)
    ss = sb("ss", [128, 4])
    ms = sb("ms", [128, 4])
    rstd = sb("rstd", [128, 4])
    ubf = sb("ubf", [128, 2, T], BF16)
    u32 = sb("u32", [128, 2, T])
    tr = sb("tr", [128, 2, T])
    ti = sb("ti", [128, 2, T])
    a_t = sb("a_t", [128, 2, T])
    a2s = sb("a2s", [128, 2, T])
    hh = sb("hh", [128, 2, T])
    tz = sb("tz", [128, 2, T])
    hst = sb("hst", [128, KC])
    yrT = sb("yrT", [128, KC, T], BF16)
    ymT = sb("ymT", [128, KC, T], BF16)
    Bc = sb("Bc", [4, 1])
    Gext = sb("Gext", [4, 5])
    dl0 = sb("dl0", [4, 4])
    dl = sb("dl", [4, 4])
    rhsd = sb("rhsd", [4, 4, 4])
    wtok = sb("wtok", [128, NSUB, 8])
    dlt = sb("dlt", [128, 16])
    cbf = sb("cbf", [128, 2, T], BF16)
    scm = sb("scm", [128, 2, T])
    qT = sb("qT", [128, 2, T], BF16)
    kT = sb("kT", [128, 2, T], BF16)
    kb = sb("kb", [128, NSUB, 256], BF16)
    wva = sb("wva", [128, NSUB, 260], BF16)
    t1sz = sb("t1sz", [128, 4, T])
    t1 = t1sz[:, 0:2, :]
    szm = t1sz[:, 2:4, :]
    c32 = t1
    to_ = sb("to_", [128, NSUB, 256])
    hmp = to_
    hn = sb("hn", [128, NSUB, 256], BF16)
    smT = sb("smT", [128, 2, 128], BF16)
    Cst = sb("Cst", [128, H + 1, 2, 260])
    Cbf = sb("Cbf", [128, 2, 2, 260], BF16)
    stats = sb("stats", [128, NSUB, 6])
    mv = sb("mv", [128, NSUB, 2])
    den = sb("den", [128, NSUB])
    epsv = sb("epsv", [128, NSUB])
    rs4 = sb("rs4", [128, NSUB])
    nmr = sb("nmr", [128, NSUB])
    trm = to_[:].rearrange("p (a b) c -> p a (b c)", a=2)
    y1 = t1sz
    y2 = sb("y2", [128, T])
    yT = sb("yT", [128, KC, T], BF16)
    obuf = sb("obuf", [128, D])
    ss2 = sb("ss2", [128, 2, 2])
    ms2 = sb("ms2", [128, 2])
    rs2 = sb("rs2", [128, 2])

    ps = es.enter_context(nc.psum_tensor("ps", [128, 8, 512], F32))

    def bank(b):
        return ps[:, b, :]

    def bank_bf(b):
        return ps[:, b, :].bitcast(BF16)

    class Rot:
        def __init__(self, banks):
            self.banks = list(banks)
            self.i = 0

        def __call__(self):
            b = self.banks[self.i % len(self.banks)]
            self.i += 1
            return b

    nb = Rot(range(8))
    nbR = Rot([0, 1])
    nbRz = Rot([2])
    nbSP = Rot([4])
    nbDC = Rot([5])
    nbM = Rot([6, 7])

    def mm(out, lhsT, rhs, start, stop, reads, writes):
        S_.add("pe", lambda e: e.matmul(out, lhsT, rhs, start=start, stop=stop), reads, writes)

    def tp(out, in_, idn, reads, writes):
        S_.add("pe", lambda e: e.transpose(out, in_, idn), reads, writes)

    def act(out, in_, func, reads, writes, bias=None, scale=None, accum=None):
        kw = {}
        if bias is not None:
            kw["bias"] = bias
        if scale is not None:
            kw["scale"] = scale
        if accum is not None:
            kw["accum_out"] = accum
        S_.add("act", lambda e: e.activation(out, in_, func, **kw), reads, writes, multi=accum is not None)

    def tt(eng, out, in0, in1, op, reads, writes):
        S_.add(eng, lambda e: e.tensor_tensor(out, in0, in1, op), reads, writes)

    def ts(eng, out, in0, s1, s2, op0, op1, reads, writes):
        if s2 is None:
            S_.add(eng, lambda e: e.tensor_scalar(out, in0, s1, None, op0), reads, writes)
        else:
            S_.add(eng, lambda e: e.tensor_scalar(out, in0, s1, s2, op0, op1), reads, writes)

    def stt(out, in0, scalar, in1, op0, op1, reads, writes):
        S_.add("dve", lambda e: e.scalar_tensor_tensor(out, in0, scalar, in1, op0, op1), reads, writes)

    def scan(out, d0, d1, init, op0, op1, reads, writes):
        S_.add("dve", lambda e: e.tensor_tensor_scan(out, d0, d1, init, op0, op1), reads, writes)

    def cp(eng, out, in_, reads, writes):
        S_.add(eng, lambda e: e.tensor_copy(out, in_), reads, writes)

    def dma(eng, out, in_, reads, writes, semkey, **kw):
        S_.add(eng, lambda e: e.dma_start(out=out, in_=in_, **kw), reads, writes, dma=True, semkey=semkey)

    def memset(eng, ap, val, writes):
        S_.add(eng, lambda e: e.memset(ap, val), (), writes)

    dma("sp", ident32[:], cid_d, (), ["ident32"], "ld_c1")
    dma("sp", maskT[:], cmask_d, (), ["maskT"], "ld_c2")
    dma("sp", pv[:], pvec_d, (), ["pv"], "ld_c3")
    dma("sp", bif[:], bif_d, (), ["bif"], "ld_c4")
    dma("sp", gpost_bc[:], gpost_d.partition_broadcast(128), (), ["gpost_bc"], "ld_c5")
    dma("pool", wif[:], wif_d, (), ["wif"], "ld_wif")
    memset("pool", ones4[:], 1.0, ["ones4"])
    memset("pool", onesrow[:], 1.0, ["onesrow"])
    memset("pool", neghalf[:], -0.5, ["neghalf"])
    memset("pool", wva[:], 0.0, ["wva"])
    memset("pool", Cbf[:], 0.0, ["Cbf"])
    memset("pool", rxb[:], 0.0, [("rxb", c) for c in range(KC)])
    memset("pool", mxb[:], 0.0, [("mxb", c) for c in range(KC)])
    cp("dve", ident[:], ident32[:], ["ident32"], ["ident"])
    for c in range(4):
        cp("dve", eye4bc[:, c, :], ident32[0:4, 0:4], ["ident32"], ["eye4bc"])
    for k in range(KC):
        cp("dve", gTbc[:, k, :], pv[:, PV_GPRE, k:k + 1].to_broadcast([128, 128]), ["pv"], ["gTbc"])
    act(ptmp[:], pv[:, PV_LAM, :], AF.Exp, ["pv"], ["ptmp"], scale=-1.0)
    act(ptmp[:], ptmp[:], AF.Ln, ["ptmp"], ["ptmp"], bias=1.0)
    ts("dve", cv1[:], ptmp[:], -8.0, None, ALU.mult, None, ["ptmp"], ["cv1"])
    ts("dve", cvh[:], ptmp[:], -4.0, None, ALU.mult, None, ["ptmp"], ["cvh"])
    ts("dve", hba[:], pv[:, PV_RBA, :], 0.5, None, ALU.mult, None, ["pv"], ["hba"])
    ts("dve", hbx[:], pv[:, PV_RBX, :], 0.5, None, ALU.mult, None, ["pv"], ["hbx"])
    ts("dve", nbf[:], bif[:, 1:2], -1.0, None, ALU.mult, None, ["bif"], ["nbf"])
    first_order = [HU_RX, HU_RZ, HU_MX, HU_MZ, HU_MO, HU_RX + 1, HU_RZ + 1, HU_MX + 1, HU_MZ + 1, HU_MO + 1]
    order = list(first_order)
    for i in (2, 3):
        order += [HU_RX + i, HU_RZ + i, HU_MX + i, HU_MZ + i, HU_MO + i]
    for i in range(4):
        order += [HU_GR + i, HU_BR + i, HU_GM + i, HU_BM + i]
    order += [HU_WO + i for i in range(4)]
    assert sorted(order) == list(range(NHU))
    small = [(wa, wgate_d[0], "wa"), (wx, wgate_d[1], "wx"), (wq, wqkv_d[0], "wq"), (wk, wqkv_d[1], "wk"),
             (wv, wqkv_d[2], "wv")]
    late_casts = order[10:]

    def emit_cast(g):
        dma("pool", wbf_d[g], wbig_d[g], (), [("wbf", g)], ("wbf", g), max_dma_last_dim=4096)

    for i, g in enumerate(order[:10]):
        emit_cast(g)
        if i in (1, 3, 5, 7, 9):
            t_, d_, k_ = small[i // 2]
            dma("pool", t_[:], d_, (), [k_], "ld_" + k_, max_dma_last_dim=4096)

    ring_free = list(range(NRING))

    def load_hu(u):
        slot = ring_free.pop(0)
        dma("sp", ring[:, slot], wbf_d[u], [("wbf", u)], [("ring", slot)], ("ring", slot))
        return slot

    def release(slot):
        ring_free.append(slot)

    def lhs_hu(slot, k, j):
        return ring[:, slot, k, j * 128:(j + 1) * 128]

    def proj_fm(slot, j, b):
        for k in range(KC):
            mm(bank(b), lhs_hu(slot, k, j), xnT[:, k, :], k == 0, k == KC - 1,
               [("ring", slot), "xnT"], [("ps", b)])

    def build_diag(dg, par, row0, c, key):
        for jj in range(4):
            ts("pool", dg[:, par, jj, :], ident[:], pv[:, row0 + jj, c:c + 1], 0.0, ALU.mult, ALU.add,
               ["ident", "pv"], [(key, par)])

    def gen_X(tok0):
        def load(sub):
            r0 = tok0 + sub * 128
            dma("sp", xt[:, sub % 2, :], x_d[r0:r0 + 128, :], (), [("xt", sub % 2)], ("xt", sub % 2))

        load(0)
        load(1)
        yield
        for sub in range(NSUB):
            slot = sub % 2
            act(xs[:], xt[:, slot, :], AF.Square, [("xt", slot)], ["xs", ("ss", sub)],
                accum=ss[:, sub:sub + 1])
            ts("dve", ms[:, sub:sub + 1], ss[:, sub:sub + 1], 1.0 / D, EPS, ALU.mult, ALU.add,
               [("ss", sub)], [("ms", sub)])
            tt("pool", rstd[:, sub:sub + 1], ms[:, sub:sub + 1], neghalf[:, 0:1], ALU.pow,
               [("ms", sub), "neghalf"], [("rstd", sub)])
            act(xs[:], xt[:, slot, :], AF.Copy, [("xt", slot), ("rstd", sub)], ["xs"],
                scale=rstd[:, sub:sub + 1])
            if sub + 2 < NSUB:
                load(sub + 2)
            b = nb()
            for k in range(KC):
                tp(bank_bf(b)[:, k * 128:(k + 1) * 128], xs[:, k * 128:(k + 1) * 128], ident[:],
                   ["xs", "ident"], [("ps", b)])
            tt("dve", xnT[:, :, sub * 128:(sub + 1) * 128],
               bank_bf(b).rearrange("p (k t) -> p k t", k=KC), gTbc[:], ALU.mult,
               [("ps", b), "gTbc"], ["xnT"])
            yield

    def gen_G(first):
        nbG = Rot([0, 1, 2, 3])
        bi_, bf_ = nbG(), nbG()
        for k in range(KC):
            mm(bank(bi_)[0:4, :], wif[:, k, 0:4], xnT[:, k, :], k == 0, k == KC - 1,
               ["wif", "xnT"], [("ps", bi_)])
        for k in range(KC):
            mm(bank(bf_)[0:4, :], wif[:, k, 4:8], xnT[:, k, :], k == 0, k == KC - 1,
               ["wif", "xnT"], [("ps", bf_)])
        if first:
            memset("dve", Bc[:], 0.0, ["Bc"])
            memset("dve", Gext[:], 0.0, ["Gext"])
            memset("dve", hst[:], 0.0, ["hst"])
            memset("dve", Cst[:], 0.0, [("Cst", h_) for h_ in range(H)] + ["Ctmp"])
            memset("pool", rxb[:, :, 0:3], 0.0, [("rxb", c) for c in range(KC)])
            memset("pool", mxb[:, :, 0:3], 0.0, [("mxb", c) for c in range(KC)])
        else:
            cp("dve", Gext[:, 0:1], Gext[:, 4:5], ["Gext"], ["Gext"])
        yield
        lfv, Btv, Atv, Gtv, E1v, E2v = (u32[0:4, 0, :], tr[0:4, 0, :], ti[0:4, 0, :], a_t[0:4, 0, :],
                                         a2s[0:4, 0, :], tz[0:4, 0, :])
        kl, kB, kA, kG, k1, k2 = ("u32", 0), ("tr", 0), ("ti", 0), ("a_t", 0), ("a2s", 0), ("tz", 0)
        c3 = "p (c t) -> p c t"
        act(lfv, bank(bf_)[0:4, :], AF.Exp, [("ps", bf_), "nbf"], [kl], bias=nbf[:, 0:1], scale=-1.0)
        act(lfv, lfv, AF.Ln, [kl], [kl], bias=1.0)
        yield
        scan(Btv, onesrow[:], lfv, Bc[:, 0:1], ALU.mult, ALU.subtract, ["onesrow", kl, "Bc"], [kB])
        cp("dve", Bc[:], Btv[:, T - 1:T], [kB], ["Bc"])
        stt(Atv, bank(bi_)[0:4, :], bif[:, 0:1], Btv, ALU.add, ALU.subtract, [("ps", bi_), "bif", kB], [kA])
        scan(Gtv, Atv, Atv, Gext[:, 0:1], ALU.max, ALU.max, [kA, "Gext"], [kG])
        Gv = Gtv.rearrange(c3, t=128)
        cp("dve", Gext[:, 1:5].unsqueeze(2), Gv[:, :, 127:128], [kG], ["Gext"])
        tt("dve", dl0[:], Gext[:, 0:4], Gext[:, 1:5], ALU.subtract, ["Gext"], ["dl0"])
        Rb = Gext[:, 1:5].unsqueeze(2).to_broadcast([4, 4, 128])
        tt("dve", E1v.rearrange(c3, t=128), Atv.rearrange(c3, t=128), Rb, ALU.subtract, [kA, "Gext"], [k1])
        tt("dve", E2v.rearrange(c3, t=128), Btv.rearrange(c3, t=128), Rb, ALU.add, [kB, "Gext"], [k2])
        yield
        act(dl[:], dl0[:], AF.Exp, ["dl0"], ["dl"])
        act(E1v, E1v, AF.Exp, [k1], [k1])
        act(E2v, E2v, AF.Exp, [k2], [k2], scale=-1.0)
        tt("dve", rhsd[:], dl[:].unsqueeze(2).to_broadcast([4, 4, 4]), eye4bc[:], ALU.mult,
           ["dl", "eye4bc"], ["rhsd"])
        yield
        bt_ = nbG()
        for c in range(NSUB):
            mm(bank(bt_)[:, c * 8:c * 8 + 4], E1v[:, c * 128:(c + 1) * 128], ident32[0:4, 0:4], True, True,
               [k1, "ident32"], [("ps", bt_)])
            mm(bank(bt_)[:, c * 8 + 4:c * 8 + 8], E2v[:, c * 128:(c + 1) * 128], ident32[0:4, 0:4], True, True,
               [k2, "ident32"], [("ps", bt_)])
        bd_ = nbG()
        mm(bank(bd_)[:, 0:16], ones4[:], rhsd[:].rearrange("p c h -> p (c h)"), True, True,
           ["ones4", "rhsd"], [("ps", bd_)])
        yield
        cp("dve", wtok[:].rearrange("p c j -> p (c j)"), bank(bt_)[:, 0:32], [("ps", bt_)], ["wtok"])
        cp("dve", dlt[:], bank(bd_)[:, 0:16], [("ps", bd_)], ["dlt"])
        yield

    def gen_R(pre):
        s_rx, s_rz = pre
        for m in range(KC // 2):
            if m > 0:
                s_rx = load_hu(HU_RX + m)
                s_rz = load_hu(HU_RZ + m)
            bA, bB, bC, bD = (0, 1, 2, 3) if m % 2 == 0 else (2, 3, 0, 1)
            cs = (2 * m, 2 * m + 1)
            for p, c in enumerate(cs):
                build_diag(dgR, p, PV_RCW, c, "dgR")
                b_rx = (bA, bB)[p]
                proj_fm(s_rx, p, b_rx)
                cp("dve", rxb[:, c, 3:3 + T], bank(b_rx), [("ps", b_rx)], [("rxb", c)])
            yield
            for p, c in enumerate(cs):
                b_u = (bC, bD)[p]
                for jj in range(4):
                    mm(bank(b_u), dgR[:, p, jj, :], rxb[:, c, jj:jj + T], jj == 0, jj == 3,
                       [("dgR", p), ("rxb", c)], [("ps", b_u)])
                act(ubf[:, p, :], bank(b_u), AF.Identity, [("ps", b_u), "pv"], [("ubf", p)],
                    bias=pv[:, PV_RCB, c:c + 1])
                act(u32[:, p, :], bank(b_u), AF.Identity, [("ps", b_u), "pv"], [("u32", p)],
                    bias=pv[:, PV_RCB, c:c + 1])
                cp("pool", rxb[:, c, 0:3], rxb[:, c, T:T + 3], [("rxb", c)], [("rxb", c)])
            yield
            gb = ((bA, bB), (bC, bD))
            for p, c in enumerate(cs):
                b_a, b_x = gb[p]
                mm(bank(b_a), wa[:, c, :], ubf[:, p, :], True, True, ["wa", ("ubf", p)], [("ps", b_a)])
                mm(bank(b_x), wx[:, c, :], ubf[:, p, :], True, True, ["wx", ("ubf", p)], [("ps", b_x)])
            for p, c in enumerate(cs):
                b_a, b_x = gb[p]
                act(tr[:, p, :], bank(b_a), AF.Tanh, [("ps", b_a), "hba"], [("tr", p)], bias=hba[:, c:c + 1], scale=0.5)
                act(ti[:, p, :], bank(b_x), AF.Tanh, [("ps", b_x), "hbx"], [("ti", p)], bias=hbx[:, c:c + 1], scale=0.5)
            for p, c in enumerate(cs):
                act(a_t[:, p, :], tr[:, p, :], AF.Exp, [("tr", p), "cvh"], [("a_t", p)],
                    bias=cvh[:, c:c + 1], scale=cvh[:, c:c + 1])
                act(a2s[:, p, :], tr[:, p, :], AF.Exp, [("tr", p), "cv1"], [("a2s", p)],
                    bias=cv1[:, c:c + 1], scale=cv1[:, c:c + 1])
            yield
            for p, c in enumerate(cs):
                act(a2s[:, p, :], a2s[:, p, :], AF.Sqrt, [("a2s", p)], [("a2s", p)], bias=1.0, scale=-1.0)
            for p, c in enumerate(cs):
                stt(u32[:, p, :], ti[:, p, :], 1.0, u32[:, p, :], ALU.add, ALU.mult, [("ti", p), ("u32", p)], [("u32", p)])
            yield
            for p, c in enumerate(cs):
                tt("dve", a2s[:, p, :], a2s[:, p, :], u32[:, p, :], ALU.mult, [("a2s", p), ("u32", p)], [("a2s", p)])
                scan(hh[:, p, :], a_t[:, p, :], a2s[:, p, :], hst[:, c:c + 1], ALU.mult, ALU.add,
                     [("a_t", p), ("a2s", p), "hst"], [("hh", p)])
                cp("dve", hst[:, c:c + 1], hh[:, p, T - 1:T], [("hh", p)], ["hst"])
            yield
            for p, c in enumerate(cs):
                b_rz = (bA, bB)[p]
                proj_fm(s_rz, p, b_rz)
                act(tz[:, p, :], bank(b_rz), AF.Tanh, [("ps", b_rz)], [("tz", p)], scale=0.5)
            release(s_rx)
            release(s_rz)
            yield
            for p, c in enumerate(cs):
                b_rz = (bA, bB)[p]
                stt(hh[:, p, :], bank(b_rz), 0.25, hh[:, p, :], ALU.mult, ALU.mult, [("ps", b_rz), ("hh", p)], [("hh", p)])
                stt(yrT[:, c, :], tz[:, p, :], 1.0, hh[:, p, :], ALU.add, ALU.mult, [("tz", p), ("hh", p)], [("yrT", c)])
            yield

    def gen_M(pre):
        s_mx, s_mz, s_mo = pre
        for h_ in range(H):
            if h_ > 0:
                s_mx, s_mz, s_mo = nxt_slots
            for e2 in range(2):
                c = 2 * h_ + e2
                build_diag(dgM, e2, PV_MCW, c, "dgM")
                b_mx = nbM()
                proj_fm(s_mx, e2, b_mx)
                cp("dve", mxb[:, c, 3:3 + T], bank(b_mx), [("ps", b_mx)], [("mxb", c)])
            release(s_mx)
            yield "dense"
            for e2 in range(2):
                c = 2 * h_ + e2
                b_c = nbM()
                for jj in range(4):
                    mm(bank(b_c), dgM[:, e2, jj, :], mxb[:, c, jj:jj + T], jj == 0, jj == 3,
                       [("dgM", e2), ("mxb", c)], [("ps", b_c)])
                act(c32[:, e2, :], bank(b_c), AF.Silu, [("ps", b_c), "pv"], [("t1", e2)],
                    bias=pv[:, PV_MCB, c:c + 1])
                act(cbf[:, e2, :], c32[:, e2, :], AF.Copy, [("t1", e2)], [("cbf", e2)])
                act(scm[:, e2, :], c32[:, e2, :], AF.Copy, [("t1", e2), "pv"], [("scm", e2)],
                    scale=pv[:, PV_MSK, c:c + 1])
                b_mz = nbM()
                proj_fm(s_mz, e2, b_mz)
                act(szm[:, e2, :], bank(b_mz), AF.Silu, [("ps", b_mz)], [("szm", e2)])
            release(s_mz)
            yield "dense"
            for e2 in range(2):
                b_q = nbM()
                for kc in range(2):
                    mm(bank(b_q), wq[:, h_ * 2 + kc, e2 * 128:(e2 + 1) * 128], cbf[:, kc, :], kc == 0, kc == 1,
                       ["wq", ("cbf", 0), ("cbf", 1)], [("ps", b_q)])
                cp("dve", qT[:, e2, :], bank(b_q), [("ps", b_q)], [("qT", e2)])
                b_k = nbM()
                for kc in range(2):
                    mm(bank(b_k), wk[:, h_ * 2 + kc, e2 * 128:(e2 + 1) * 128], cbf[:, kc, :], kc == 0, kc == 1,
                       ["wk", ("cbf", 0), ("cbf", 1)], [("ps", b_k)])
                ts("dve", kT[:, e2, :], bank(b_k), 1.0 / 16.0, None, ALU.mult, None, [("ps", b_k)], [("kT", e2)])
            yield "dense"
            cp("dve", wva[:, :, 256], wtok[:, :, h_], ["wtok"], ["wva"])
            for sub in range(NSUB):
                tsl = slice(sub * 128, (sub + 1) * 128)
                b_kv = nbM()
                for kc in range(2):
                    mm(bank(b_kv)[:, 0:256], cbf[:, kc, tsl], wk[:, h_ * 2 + kc, :], kc == 0, kc == 1,
                       ["wk", ("cbf", 0), ("cbf", 1)], [("ps", b_kv)])
                for kc in range(2):
                    mm(bank(b_kv)[:, 256:512], mxb[:, 2 * h_ + kc, 3 + sub * 128:3 + (sub + 1) * 128],
                       wv[:, h_ * 2 + kc, :], kc == 0, kc == 1,
                       ["wv", ("mxb", 2 * h_), ("mxb", 2 * h_ + 1)], [("ps", b_kv)])
                act(kb[:, sub, :], bank(b_kv)[:, 0:256], AF.Copy, [("ps", b_kv)], ["kb"], scale=1.0 / 16.0)
                act(wva[:, sub, 0:256], bank(b_kv)[:, 256:512], AF.Copy, [("ps", b_kv), "wtok"], ["wva"],
                    scale=wtok[:, sub, h_:h_ + 1])
                b_o = nbM()
                for k in range(KC):
                    mm(bank(b_o)[:, 0:256], xnT[:, k, tsl], ring[:, s_mo, k, :],
                       k == 0, k == KC - 1, ["xnT", ("ring", s_mo)], [("ps", b_o)])
                act(to_[:, sub, :], bank(b_o)[:, 0:256], AF.Tanh, [("ps", b_o)], [("to_", sub)], scale=0.5)
                yield "dense"
            release(s_mo)
            for e2 in range(2):
                c = 2 * h_ + e2
                cp("pool", mxb[:, c, 0:3], mxb[:, c, T:T + 3], [("mxb", c)], [("mxb", c)])
            if h_ + 1 < H:
                nxt_slots = (load_hu(HU_MX + h_ + 1), load_hu(HU_MZ + h_ + 1), load_hu(HU_MO + h_ + 1))
            for sub in range(NSUB):
                tsl = slice(sub * 128, (sub + 1) * 128)
                par = sub % 2
                hc = h_ if par == 0 else H
                hn_ = H if par == 0 else h_
                dsc = dlt[:, sub * 4 + h_:sub * 4 + h_ + 1]
                skeys = [("Cst", h_), "Ctmp"]
                act(Cbf[:, par, :, 0:257], Cst[:, hc, :, 0:257], AF.Copy, skeys + ["dlt"], [("Cbf", par)], scale=dsc)
                b_sp = nbSP()
                for kc in range(2):
                    mm(bank(b_sp)[:, 0:128], kT[:, kc, tsl], qT[:, kc, tsl], kc == 0, kc == 1,
                       [("kT", 0), ("kT", 1), ("qT", 0), ("qT", 1)], [("ps", b_sp)])
                b_dc = nbDC()
                for kc in range(2):
                    mm(bank(b_dc)[:, kc * 256:(kc + 1) * 256], kb[:, sub, kc * 128:(kc + 1) * 128],
                       wva[:, sub, 0:256], True, True, ["kb", "wva"], [("ps", b_dc)])
                yield "lat"
                tt("dve", smT[:, par, :], bank(b_sp)[:, 0:128], maskT[:], ALU.mult,
                   [("ps", b_sp), "maskT"], [("smT", par)])
                yield "lat"
                Pv = bank(b_sp)[:, 128:385]
                mm(Pv, smT[:, par, :], wva[:, sub, 0:257], True, False, [("smT", par), "wva"], [("ps", b_sp)])
                for kc in range(2):
                    mm(Pv, qT[:, kc, tsl], Cbf[:, par, kc, 0:257], False, kc == 1,
                       [("qT", 0), ("qT", 1), ("Cbf", par)], [("ps", b_sp)])
                for kc in range(2):
                    mm(bank(b_sp)[:, 400 + 2 * kc:402 + 2 * kc], kb[:, sub, kc * 128:(kc + 1) * 128],
                       wva[:, sub, 256:258], True, True, ["kb", "wva"], [("ps", b_sp)])
                yield "lat"
                stt(Cst[:, hn_, :, 0:256], Cst[:, hc, :, 0:256], dsc,
                    bank(b_dc).rearrange("p (k e) -> p k e", k=2), ALU.mult, ALU.add,
                    skeys + ["dlt", ("ps", b_dc)], skeys)
                stt(Cst[:, hn_, :, 256], Cst[:, hc, :, 256], dsc,
                    bank(b_sp)[:, 400:404].rearrange("p (k t) -> p k t", t=2)[:, :, 0], ALU.mult, ALU.add,
                    skeys + ["dlt", ("ps", b_sp)], skeys)
                stt(hmp[:, sub, :], to_[:, sub, :], 1.0, Pv[:, 0:256], ALU.add, ALU.mult,
                    [("to_", sub), ("ps", b_sp)], [("to_", sub)])
                ts("dve", den[:, sub:sub + 1], Pv[:, 256:257], -1.0, wtok[:, sub, 4 + h_:5 + h_],
                   ALU.mult, ALU.max, [("ps", b_sp), "wtok"], [("den", sub)])
                tt("dve", den[:, sub:sub + 1], den[:, sub:sub + 1], Pv[:, 256:257], ALU.max,
                   [("ps", b_sp), ("den", sub)], [("den", sub)])
                S_.add("dve", (lambda o, i: (lambda e: e.bn_stats(o, i)))(stats[:, sub, :], hmp[:, sub, :]),
                       [("to_", sub)], [("stats", sub)])
                S_.add("dve", (lambda o, i: (lambda e: e.bn_aggr(o, i)))(mv[:, sub, :], stats[:, sub, :]),
                       [("stats", sub)], [("mv", sub)])
                yield "lat"
            allsub = list(range(NSUB))
            tt("dve", epsv[:], den[:], den[:], ALU.mult, [("den", s) for s in allsub], ["epsv"])
            stt(epsv[:], epsv[:], 4.0 * EPS, mv[:, :, 1], ALU.mult, ALU.add,
                ["epsv"] + [("mv", s) for s in allsub], ["epsv"])
            tt("pool", rs4[:], epsv[:], neghalf[:], ALU.pow, ["epsv", "neghalf"], ["rs4"])
            stt(nmr[:], mv[:, :, 0], -1.0, rs4[:], ALU.mult, ALU.mult, [("mv", s) for s in allsub] + ["rs4"], ["nmr"])
            yield "lat"
            b_t = nbM()
            for sub in range(NSUB):
                act(hn[:, sub, :], hmp[:, sub, :], AF.Identity, [("to_", sub), "rs4", "nmr"], [("hn", sub)],
                    bias=nmr[:, sub:sub + 1], scale=rs4[:, sub:sub + 1])
                for e2 in range(2):
                    tp(bank_bf(b_t)[:, e2 * 512 + sub * 128:e2 * 512 + (sub + 1) * 128],
                       hn[:, sub, e2 * 128:(e2 + 1) * 128], ident[:], [("hn", sub), "ident"], [("ps", b_t)])
            yield "lat"
            for e2 in range(2):
                c = 2 * h_ + e2
                stt(t1[:, e2, :], bank_bf(b_t)[:, e2 * 512:(e2 + 1) * 512], pv[:, PV_MNG, c:c + 1], scm[:, e2, :],
                    ALU.mult, ALU.add, [("ps", b_t), "pv", ("scm", e2)], [("t1", e2)])
                tt("dve", ymT[:, c, :], t1[:, e2, :], szm[:, e2, :], ALU.mult, [("t1", e2), ("szm", e2)], [("ymT", c)])
            yield "lat"

    def gen_F(tile, tok0):
        for q in range(4):
            s_gr = load_hu(HU_GR + q)
            s_br = load_hu(HU_BR + q)
            s_gm = load_hu(HU_GM + q)
            s_bm = load_hu(HU_BM + q)
            for j in range(2):
                b_g = nb()
                proj_fm(s_gr, j, b_g)
                act(trm[:, j, :], bank(b_g), AF.Tanh, [("ps", b_g)], [("to_", 2 * j), ("to_", 2 * j + 1)], scale=0.5)
                b_r = nb()
                for k in range(KC):
                    mm(bank(b_r), lhs_hu(s_br, k, j), yrT[:, k, :], k == 0, k == KC - 1,
                       [("ring", s_br), ("yrT", k)], [("ps", b_r)])
                stt(y1[:, j, :], trm[:, j, :], 1.0, bank(b_r), ALU.add, ALU.mult,
                    [("to_", 2 * j), ("to_", 2 * j + 1), ("ps", b_r)], [("t1", j)])
            release(s_gr)
            release(s_br)
            for j in range(2):
                dc = q * 2 + j
                b_g = nb()
                proj_fm(s_gm, j, b_g)
                act(trm[:, j, :], bank(b_g), AF.Tanh, [("ps", b_g)], [("to_", 2 * j), ("to_", 2 * j + 1)], scale=0.5)
                b_m = nb()
                for k in range(KC):
                    mm(bank(b_m), lhs_hu(s_bm, k, j), ymT[:, k, :], k == 0, k == KC - 1,
                       [("ring", s_bm), ("ymT", k)], [("ps", b_m)])
                stt(y2[:], trm[:, j, :], 1.0, bank(b_m), ALU.add, ALU.mult,
                    [("to_", 2 * j), ("to_", 2 * j + 1), ("ps", b_m)], ["y2"])
                tt("dve", yT[:, dc, :], y1[:, j, :], y2[:], ALU.add, [("t1", j), "y2"], [("yT", dc)])
            release(s_gm)
            release(s_bm)
        s_o = [load_hu(HU_WO + q) for q in range(4)]
        yield
        for sub in range(NSUB):
            tsl = slice(sub * 128, (sub + 1) * 128)
            osl = sub % 2
            r0 = tok0 + sub * 128
            bo = [nb(), nb()]
            for hf in range(2):
                for q2 in range(2):
                    q = hf * 2 + q2
                    for k in range(KC):
                        mm(bank(bo[hf])[:, q2 * 256:(q2 + 1) * 256], yT[:, k, tsl], ring[:, s_o[q], k, :],
                           k == 0, k == KC - 1, [("yT", k), ("ring", s_o[q])], [("ps", bo[hf])])
                act(y2[:], bank(bo[hf]), AF.Square, [("ps", bo[hf])], ["y2", ("ss2", osl, hf)],
                    accum=ss2[:, osl, hf:hf + 1])
            tt("dve", ms2[:, osl:osl + 1], ss2[:, osl, 0:1], ss2[:, osl, 1:2], ALU.add,
               [("ss2", osl, 0), ("ss2", osl, 1)], [("ms2", osl)])
            ts("dve", ms2[:, osl:osl + 1], ms2[:, osl:osl + 1], 1.0 / D, 4.0 * EPS, ALU.mult, ALU.add,
               [("ms2", osl)], [("ms2", osl)])
            tt("pool", rs2[:, osl:osl + 1], ms2[:, osl:osl + 1], neghalf[:, 0:1], ALU.pow,
               [("ms2", osl), "neghalf"], [("rs2", osl)])
            xr = t1sz[:, 0:2, :].rearrange("p a t -> p (a t)")
            xrk = [("t1", 0), ("t1", 1)]
            dma("sp", xr, x_d[r0:r0 + 128, :], (), xrk, "xr")
            for hf in range(2):
                stt(obuf[:, hf * 512:(hf + 1) * 512], bank(bo[hf]), rs2[:, osl:osl + 1],
                    gpost_bc[:, hf * 512:(hf + 1) * 512], ALU.mult, ALU.mult,
                    [("ps", bo[hf]), ("rs2", osl), "gpost_bc"], ["obuf"])
            tt("pool", obuf[:], obuf[:], xr, ALU.add, ["obuf"] + xrk, ["obuf"])
            dma("sp", out_d[r0:r0 + 128, :], obuf[:], ["obuf"], [("out", tile, sub)], ("st", 0))
            yield
        for q in range(4):
            release(s_o[q])

    NT_ALL = NSEQ * NTILE

    def tile_tok0(tile):
        seq, tl = divmod(tile, NTILE)
        return seq * S + tl * T

    for _ in gen_X(tile_tok0(0)):
        pass
    preR = (load_hu(HU_RX), load_hu(HU_RZ))
    preM = (load_hu(HU_MX), load_hu(HU_MZ), load_hu(HU_MO))
    for tile in range(NT_ALL):
        first = tile % NTILE == 0
        tok0 = tile_tok0(tile)
        gG = gen_G(first)
        gM = gen_M(preM)
        for _ in range(3):
            next(gM)
            if late_casts:
                emit_cast(late_casts.pop(0))
            next(gG, None)
            next(gG, None)
        for _ in gG:
            pass
        gR = gen_R(preR)
        r_alive = True
        nlat = 0
        for tag in gM:
            if late_casts:
                emit_cast(late_casts.pop(0))
            if tag == "lat" and r_alive:
                nlat += 1
                if nlat % 2:
                    continue
                try:
                    next(gR)
                except StopIteration:
                    r_alive = False
        if r_alive:
            for _ in gR:
                pass
        while late_casts:
            emit_cast(late_casts.pop(0))
        gF = gen_F(tile, tok0)
        next(gF)
        if tile + 1 < NT_ALL:
            preR = (load_hu(HU_RX), load_hu(HU_RZ))
            preM = (load_hu(HU_MX), load_hu(HU_MZ), load_hu(HU_MO))
            gX = gen_X(tile_tok0(tile + 1))
            next(gX)
            for _ in gF:
                next(gX, None)
            for _ in gX:
                pass
        else:
            for _ in gF:
                pass
    S_.emit(nc, es, [k for k in [("st", 0)] if k in S_.dma_count])
    es.close()
    return nc


def prep_weights(inp):
    f = np.float32
    w_in = np.asarray(inp["w_in"][0], f)
    cols = {}
    offs = [0, 1024, 2048, 3072, 4096, 5120, 5124, 5128, 6152, 7176]
    names = ["r_x", "r_z", "m_x", "m_z", "m_o", "m_i", "m_f", "gate_r", "gate_m"]
    for n, a, b in zip(names, offs[:-1], offs[1:]):
        cols[n] = w_in[:, a:b]
    big = [cols["r_x"], cols["r_z"], cols["m_x"], cols["m_z"], cols["m_o"], cols["gate_r"], cols["gate_m"],
           np.asarray(inp["w_branch_r"][0], f), np.asarray(inp["w_branch_m"][0], f), np.asarray(inp["w_out"][0], f)]
    units = []
    for W in big:
        for q in range(4):
            Wh = W[:, q * 256:(q + 1) * 256]
            units.append(Wh.reshape(8, 128, 256).transpose(1, 0, 2))
    wbig = np.ascontiguousarray(np.stack(units, 0))
    qkv = []
    for n in ("ml_w_q", "ml_w_k", "ml_w_v"):
        W = np.asarray(inp[n][0], f)
        qkv.append(W.reshape(4, 2, 128, 256).transpose(2, 0, 1, 3).reshape(128, 8, 256))
    wqkv = np.ascontiguousarray(np.stack(qkv, 0))
    gates = []
    for n in ("rg_w_a", "rg_w_x"):
        W = np.asarray(inp[n][0], f)
        G = np.zeros((128, 8, 128), f)
        for c in range(8):
            G[0:64, c, 0:64] = W[2 * c]
            G[64:128, c, 64:128] = W[2 * c + 1]
        gates.append(G)
    wgate = np.ascontiguousarray(np.stack(gates, 0))
    wif = np.ascontiguousarray(w_in[:, 5120:5128].reshape(8, 128, 8).transpose(1, 0, 2))
    pnames = ["g_pre", "rg_conv_b", "rg_b_a", "rg_b_x", "rg_lambda", "ml_conv_b", "ml_norm_g", "ml_skip"]
    prow = [np.asarray(inp[n][0], f) for n in pnames]
    prow += [np.asarray(inp["rg_conv_w"][0], f)[j] for j in range(4)]
    prow += [np.asarray(inp["ml_conv_w"][0], f)[j] for j in range(4)]
    pvec = np.ascontiguousarray(np.stack([r.reshape(8, 128).T for r in prow], 1))
    bif = np.ascontiguousarray(np.stack([np.asarray(inp["ml_b_i"][0], f), np.asarray(inp["ml_b_f"][0], f)], 1))
    gpost = np.ascontiguousarray(np.asarray(inp["g_post"][0], f).reshape(1, 1024))
    c_ident = np.eye(128, dtype=f)
    c_mask = np.triu(np.ones((128, 128), f))
    return dict(wbig=wbig, wqkv=wqkv, wgate=wgate, wif=wif, pvec=pvec, bif=bif, gpost=gpost,
                c_ident=c_ident, c_mask=c_mask)


_NC_CACHE = {}


def kernel(**inputs):
    x = np.asarray(inputs["x"], np.float32)
    B, S, _ = x.shape
    ncores = 8
    nseq = B // ncores
    key = (nseq, S)
    if key not in _NC_CACHE:
        _NC_CACHE[key] = build(nseq, S)
    nc = _NC_CACHE[key]
    wts = prep_weights(inputs)
    in_maps = []
    for c in range(ncores):
        m = dict(wts)
        m["x"] = np.ascontiguousarray(x[c * nseq:(c + 1) * nseq].reshape(nseq * S, D))
        in_maps.append(m)
    res = run_bass_kernel_spmd(nc, in_maps, core_ids=list(range(ncores)))
    outs = [np.asarray(r["out"], np.float32).reshape(nseq, S, D) for r in res.results]
    return np.concatenate(outs, 0)
```

```python
import numpy as np
from contextlib import ExitStack
import concourse.bass as bass
import concourse.mybir as mybir
from concourse.bass_utils import run_bass_kernel_spmd

F32 = mybir.dt.float32
BF16 = mybir.dt.bfloat16
ALU = mybir.AluOpType
AF = mybir.ActivationFunctionType

D = 1024
KC = 8
T = 512
NSUB = 4
H = 4
EPS = 1e-6
NHU = 40
NRING = 9
HU_RX, HU_RZ, HU_MX, HU_MZ, HU_MO, HU_GR, HU_GM, HU_BR, HU_BM, HU_WO = 0, 4, 8, 12, 16, 20, 24, 28, 32, 36
PV_GPRE, PV_RCB, PV_RBA, PV_RBX, PV_LAM, PV_MCB, PV_MNG, PV_MSK = range(8)
PV_RCW = 8
PV_MCW = 12
NPV = 16


class _Op:
    __slots__ = ("eng", "fn", "deps", "dma", "semkey", "sigval", "signalled", "dmaval", "multi")

    def __init__(self, eng, fn, dma, semkey):
        self.multi = False
        self.eng = eng
        self.fn = fn
        self.deps = []
        self.dma = dma
        self.semkey = semkey
        self.sigval = 0
        self.signalled = False
        self.dmaval = 0


class Sched:
    ENGS = ("pe", "act", "dve", "pool", "sp")

    def __init__(self):
        self.ops = []
        self.last_writer = {}
        self.readers = {}
        self.dma_count = {}

    def add(self, eng, fn, reads=(), writes=(), dma=False, semkey=None, multi=False):
        op = _Op(eng, fn, dma, semkey)
        op.multi = multi
        deps = {}
        for r in reads:
            w = self.last_writer.get(r)
            if w is not None:
                deps[id(w)] = (w, True)
            if isinstance(r, tuple) and r[0] == "ps":
                for rd in self.readers.get(r, ()):
                    if rd.eng != eng and id(rd) not in deps:
                        deps[id(rd)] = (rd, False)
        for k in writes:
            w = self.last_writer.get(k)
            if w is not None and id(w) not in deps:
                deps[id(w)] = (w, False)
            for rd in self.readers.get(k, ()):
                if id(rd) not in deps:
                    deps[id(rd)] = (rd, False)
        for d, raw in deps.values():
            if d is op:
                continue
            if d.dma:
                op.deps.append(d)
            elif d.eng != eng:
                op.deps.append(d)
            elif eng != "pe" and not dma:
                op.deps.append(d)
            elif dma and not d.dma:
                op.deps.append(d)
        for k in writes:
            self.last_writer[k] = op
            self.readers[k] = []
        for r in reads:
            if r not in writes:
                self.readers.setdefault(r, []).append(op)
        if dma:
            n = self.dma_count.get(semkey, 0) + 1
            self.dma_count[semkey] = n
            op.dmaval = 16 * n
        self.ops.append(op)
        return op

    def finalize(self):
        for op in self.ops:
            for d in op.deps:
                if not d.dma:
                    d.signalled = True
        cnt = {e: 0 for e in self.ENGS}
        for op in self.ops:
            if op.signalled and not op.dma:
                cnt[op.eng] += 1
                op.sigval = cnt[op.eng]

    def emit(self, nc, es, final_dma_keys):
        self.finalize()
        esem = {e: es.enter_context(nc.semaphore("sem_" + e)) for e in self.ENGS}
        dsem = {}
        for k in self.dma_count:
            dsem[k] = es.enter_context(nc.semaphore("dsem_%d" % len(dsem)))
        block = es.enter_context(nc.Block())
        per = {e: [op for op in self.ops if op.eng == e] for e in self.ENGS}

        def run(eng_name, eobj):
            seen = {}
            for op in per[eng_name]:
                need = {}
                for d in op.deps:
                    if d.dma:
                        s, v = dsem[d.semkey], d.dmaval
                    else:
                        s, v = esem[d.eng], d.sigval
                    if seen.get(id(s), 0) < v and need.get(id(s), (None, 0))[1] < v:
                        need[id(s)] = (s, v)
                need = list(need.values())
                embed = None
                if need and eng_name in ("act", "dve", "pool") and not op.dma and not op.multi:
                    embed = need.pop()
                for s, v in need:
                    eobj.wait_ge(s, v)
                    seen[id(s)] = v
                inst = op.fn(eobj)
                if embed is not None:
                    inst._wait_ge(embed[0], embed[1])
                    seen[id(embed[0])] = embed[1]
                if op.dma:
                    inst.then_inc(dsem[op.semkey], 16)
                elif op.signalled:
                    inst.then_inc(esem[op.eng], 1)
            if eng_name == "sp":
                for k in final_dma_keys:
                    eobj.wait_ge(dsem[k], 16 * self.dma_count[k])

        @block.tensor
        def _(e):
            run("pe", e)

        @block.scalar
        def _(e):
            run("act", e)

        @block.vector
        def _(e):
            run("dve", e)

        @block.gpsimd
        def _(e):
            run("pool", e)

        @block.sync
        def _(e):
            run("sp", e)


def build(NSEQ, S):
    NTOK = NSEQ * S
    NTILE = S // T
    nc = bass.Bass("TRN2", target_bir_lowering=False)
    S_ = Sched()
    es = ExitStack()

    def dram(name, shape, dt, kind):
        return nc.dram_tensor(name, list(shape), dt, kind=kind).ap()

    x_d = dram("x", [NTOK, D], F32, "ExternalInput")
    wbig_d = dram("wbig", [NHU, 128, KC, 256], F32, "ExternalInput")
    wqkv_d = dram("wqkv", [3, 128, 8, 256], F32, "ExternalInput")
    wgate_d = dram("wgate", [2, 128, 8, 128], F32, "ExternalInput")
    wif_d = dram("wif", [128, 8, 8], F32, "ExternalInput")
    pvec_d = dram("pvec", [128, NPV, 8], F32, "ExternalInput")
    bif_d = dram("bif", [4, 2], F32, "ExternalInput")
    gpost_d = dram("gpost", [1, D], F32, "ExternalInput")
    cid_d = dram("c_ident", [128, 128], F32, "ExternalInput")
    cmask_d = dram("c_mask", [128, 128], F32, "ExternalInput")
    out_d = dram("out", [NTOK, D], F32, "ExternalOutput")
    wbf_d = dram("wbf", [NHU, 128, KC, 256], BF16, "Internal")

    def sb(name, shape, dt=F32):
        return es.enter_context(nc.sbuf_tensor("s_" + name, list(shape), dt))

    ring = sb("ring", [128, NRING, KC, 256], BF16)
    xt = sb("xt", [128, 2, D])
    xs = sb("xs", [128, D], BF16)
    xnT = sb("xnT", [128, KC, T], BF16)
    gTbc = sb("gTbc", [128, KC, 128])
    gpost_bc = sb("gpost_bc", [128, D])
    ident = sb("ident", [128, 128], BF16)
    ident32 = sb("ident32", [128, 128])
    maskT = sb("maskT", [128, 128])
    ones4 = sb("ones4", [4, 128])
    onesrow = sb("onesrow", [4, T])
    neghalf = sb("neghalf", [128, 4])
    eye4bc = sb("eye4bc", [4, 4, 4])
    pv = sb("pv", [128, NPV, 8])
    cv1 = sb("cv1", [128, 8])
    cvh = sb("cvh", [128, 8])
    hba = sb("hba", [128, 8])
    hbx = sb("hbx", [128, 8])
    ptmp = sb("ptmp", [128, 8])
    bif = sb("bif", [4, 2])
    nbf = sb("nbf", [4, 1])
    wq = sb("wq", [128, 8, 256], BF16)
    wk = sb("wk", [128, 8, 256], BF16)
    wv = sb("wv", [128, 8, 256], BF16)
    wa = sb("wa", [128, 8, 128], BF16)
    wx = sb("wx", [128, 8, 128], BF16)
    wif = sb("wif", [128, 8, 8], BF16)
    dgR = sb("dgR", [128, 2, 4, 128], BF16)
    dgM = sb("dgM", [128, 2, 4, 128], BF16)
    rxb = sb("rxb", [128, KC, 3 + T], BF16)
    mxb = sb("mxb", [128, KC, 3 + T], BF16)
    ss = sb("ss", [128, 4])
    ms = sb("ms", [128, 4])
    rstd = sb("rstd", [128, 4])
    ubf = sb("ubf", [128, 2, T], BF16)
    u32 = sb("u32", [128, 2, T])
    tr = sb("tr", [128, 2, T])
    ti = sb("ti", [128, 2, T])
    a_t = sb("a_t", [128, 2, T])
    a2s = sb("a2s", [128, 2, T])
    hh = sb("hh", [128, 2, T])
    tz = sb("tz", [128, 2, T])
    hst = sb("hst", [128, KC])
    yrT = sb("yrT", [128, KC, T], BF16)
    ymT = sb("ymT", [128, KC, T], BF16)
    Bc = sb("Bc", [4, 1])
    Gext = sb("Gext", [4, 5])
    dl0 = sb("dl0", [4, 4])
    dl = sb("dl", [4, 4])
    rhsd = sb("rhsd", [4, 4, 4])
    wtok = sb("wtok", [128, NSUB, 8])
    dlt = sb("dlt", [128, 16])
    cbf = sb("cbf", [128, 2, T], BF16)
    scm = sb("scm", [128, 2, T])
    qT = sb("qT", [128, 2, T], BF16)
    kT = sb("kT", [128, 2, T], BF16)
    kb = sb("kb", [128, NSUB, 256], BF16)
    wva = sb("wva", [128, NSUB, 260], BF16)
    t1sz = sb("t1sz", [128, 4, T])
    t1 = t1sz[:, 0:2, :]
    szm = t1sz[:, 2:4, :]
    c32 = t1
    to_ = sb("to_", [128, NSUB, 256])
    hmp = to_
    hn = sb("hn", [128, NSUB, 256], BF16)
    smT = sb("smT", [128, 2, 128], BF16)
    Cst = sb("Cst", [128, H + 1, 2, 260])
    Cbf = sb("Cbf", [128, 2, 2, 260], BF16)
    stats = sb("stats", [128, NSUB, 6])
    mv = sb("mv", [128, NSUB, 2])
    den = sb("den", [128, NSUB])
    epsv = sb("epsv", [128, NSUB])
    rs4 = sb("rs4", [128, NSUB])
    nmr = sb("nmr", [128, NSUB])
    trm = to_[:].rearrange("p (a b) c -> p a (b c)", a=2)
    y1 = t1sz
    y2 = sb("y2", [128, T])
    yT = sb("yT", [128, KC, T], BF16)
    obuf = sb("obuf", [128, D])
    ss2 = sb("ss2", [128, 2, 2])
    ms2 = sb("ms2", [128, 2])
    rs2 = sb("rs2", [128, 2])

    ps = es.enter_context(nc.psum_tensor("ps", [128, 8, 512], F32))

    def bank(b):
        return ps[:, b, :]

    def bank_bf(b):
        return ps[:, b, :].bitcast(BF16)

    class Rot:
        def __init__(self, banks):
            self.banks = list(banks)
            self.i = 0

        def __call__(self):
            b = self.banks[self.i % len(self.banks)]
            self.i += 1
            return b

    nb = Rot(range(8))
    nbR = Rot([0, 1])
    nbRz = Rot([2])
    nbSP = Rot([4])
    nbDC = Rot([5])
    nbM = Rot([6, 7])

    def mm(out, lhsT, rhs, start, stop, reads, writes):
        S_.add("pe", lambda e: e.matmul(out, lhsT, rhs, start=start, stop=stop), reads, writes)

    def tp(out, in_, idn, reads, writes):
        S_.add("pe", lambda e: e.transpose(out, in_, idn), reads, writes)

    def act(out, in_, func, reads, writes, bias=None, scale=None, accum=None):
        kw = {}
        if bias is not None:
            kw["bias"] = bias
        if scale is not None:
            kw["scale"] = scale
        if accum is not None:
            kw["accum_out"] = accum
        S_.add("act", lambda e: e.activation(out, in_, func, **kw), reads, writes, multi=accum is not None)

    def tt(eng, out, in0, in1, op, reads, writes):
        S_.add(eng, lambda e: e.tensor_tensor(out, in0, in1, op), reads, writes)

    def ts(eng, out, in0, s1, s2, op0, op1, reads, writes):
        if s2 is None:
            S_.add(eng, lambda e: e.tensor_scalar(out, in0, s1, None, op0), reads, writes)
        else:
            S_.add(eng, lambda e: e.tensor_scalar(out, in0, s1, s2, op0, op1), reads, writes)

    def stt(out, in0, scalar, in1, op0, op1, reads, writes):
        S_.add("dve", lambda e: e.scalar_tensor_tensor(out, in0, scalar, in1, op0, op1), reads, writes)

    def scan(out, d0, d1, init, op0, op1, reads, writes):
        S_.add("dve", lambda e: e.tensor_tensor_scan(out, d0, d1, init, op0, op1), reads, writes)

    def cp(eng, out, in_, reads, writes):
        S_.add(eng, lambda e: e.tensor_copy(out, in_), reads, writes)

    def dma(eng, out, in_, reads, writes, semkey, **kw):
        S_.add(eng, lambda e: e.dma_start(out=out, in_=in_, **kw), reads, writes, dma=True, semkey=semkey)

    def memset(eng, ap, val, writes):
        S_.add(eng, lambda e: e.memset(ap, val), (), writes)

    dma("sp", ident32[:], cid_d, (), ["ident32"], "ld_c1")
    dma("sp", maskT[:], cmask_d, (), ["maskT"], "ld_c2")
    dma("sp", pv[:], pvec_d, (), ["pv"], "ld_c3")
    dma("sp", bif[:], bif_d, (), ["bif"], "ld_c4")
    dma("sp", gpost_bc[:], gpost_d.partition_broadcast(128), (), ["gpost_bc"], "ld_c5")
    dma("pool", wif[:], wif_d, (), ["wif"], "ld_wif")
    memset("pool", ones4[:], 1.0, ["ones4"])
    memset("pool", onesrow[:], 1.0, ["onesrow"])
    memset("pool", neghalf[:], -0.5, ["neghalf"])
    memset("pool", wva[:], 0.0, ["wva"])
    memset("pool", Cbf[:], 0.0, ["Cbf"])
    memset("pool", rxb[:], 0.0, [("rxb", c) for c in range(KC)])
    memset("pool", mxb[:], 0.0, [("mxb", c) for c in range(KC)])
    cp("dve", ident[:], ident32[:], ["ident32"], ["ident"])
    for c in range(4):
        cp("dve", eye4bc[:, c, :], ident32[0:4, 0:4], ["ident32"], ["eye4bc"])
    for k in range(KC):
        cp("dve", gTbc[:, k, :], pv[:, PV_GPRE, k:k + 1].to_broadcast([128, 128]), ["pv"], ["gTbc"])
    act(ptmp[:], pv[:, PV_LAM, :], AF.Exp, ["pv"], ["ptmp"], scale=-1.0)
    act(ptmp[:], ptmp[:], AF.Ln, ["ptmp"], ["ptmp"], bias=1.0)
    ts("dve", cv1[:], ptmp[:], -8.0, None, ALU.mult, None, ["ptmp"], ["cv1"])
    ts("dve", cvh[:], ptmp[:], -4.0, None, ALU.mult, None, ["ptmp"], ["cvh"])
    ts("dve", hba[:], pv[:, PV_RBA, :], 0.5, None, ALU.mult, None, ["pv"], ["hba"])
    ts("dve", hbx[:], pv[:, PV_RBX, :], 0.5, None, ALU.mult, None, ["pv"], ["hbx"])
    ts("dve", nbf[:], bif[:, 1:2], -1.0, None, ALU.mult, None, ["bif"], ["nbf"])
    first_order = [HU_RX, HU_RZ, HU_MX, HU_MZ, HU_MO, HU_RX + 1, HU_RZ + 1, HU_MX + 1, HU_MZ + 1, HU_MO + 1]
    order = list(first_order)
    for i in (2, 3):
        order += [HU_RX + i, HU_RZ + i, HU_MX + i, HU_MZ + i, HU_MO + i]
    for i in range(4):
        order += [HU_GR + i, HU_BR + i, HU_GM + i, HU_BM + i]
    order += [HU_WO + i for i in range(4)]
    assert sorted(order) == list(range(NHU))
    small = [(wa, wgate_d[0], "wa"), (wx, wgate_d[1], "wx"), (wq, wqkv_d[0], "wq"), (wk, wqkv_d[1], "wk"),
             (wv, wqkv_d[2], "wv")]
    late_casts = order[10:]

    def emit_cast(g):
        dma("pool", wbf_d[g], wbig_d[g], (), [("wbf", g)], ("wbf", g), max_dma_last_dim=4096)

    for i, g in enumerate(order[:10]):
        emit_cast(g)
        if i in (1, 3, 5, 7, 9):
            t_, d_, k_ = small[i // 2]
            dma("pool", t_[:], d_, (), [k_], "ld_" + k_, max_dma_last_dim=4096)

    ring_free = list(range(NRING))

    def load_hu(u):
        slot = ring_free.pop(0)
        dma("sp", ring[:, slot], wbf_d[u], [("wbf", u)], [("ring", slot)], ("ring", slot))
        return slot

    def release(slot):
        ring_free.append(slot)

    def lhs_hu(slot, k, j):
        return ring[:, slot, k, j * 128:(j + 1) * 128]

    def proj_fm(slot, j, b):
        for k in range(KC):
            mm(bank(b), lhs_hu(slot, k, j), xnT[:, k, :], k == 0, k == KC - 1,
               [("ring", slot), "xnT"], [("ps", b)])

    def build_diag(dg, par, row0, c, key):
        for jj in range(4):
            ts("pool", dg[:, par, jj, :], ident[:], pv[:, row0 + jj, c:c + 1], 0.0, ALU.mult, ALU.add,
               ["ident", "pv"], [(key, par)])

    def gen_X(tok0):
        def load(sub):
            r0 = tok0 + sub * 128
            dma("sp", xt[:, sub % 2, :], x_d[r0:r0 + 128, :], (), [("xt", sub % 2)], ("xt", sub % 2))

        load(0)
        load(1)
        yield
        for sub in range(NSUB):
            slot = sub % 2
            act(xs[:], xt[:, slot, :], AF.Square, [("xt", slot)], ["xs", ("ss", sub)],
                accum=ss[:, sub:sub + 1])
            ts("dve", ms[:, sub:sub + 1], ss[:, sub:sub + 1], 1.0 / D, EPS, ALU.mult, ALU.add,
               [("ss", sub)], [("ms", sub)])
            tt("pool", rstd[:, sub:sub + 1], ms[:, sub:sub + 1], neghalf[:, 0:1], ALU.pow,
               [("ms", sub), "neghalf"], [("rstd", sub)])
            act(xs[:], xt[:, slot, :], AF.Copy, [("xt", slot), ("rstd", sub)], ["xs"],
                scale=rstd[:, sub:sub + 1])
            if sub + 2 < NSUB:
                load(sub + 2)
            b = nb()
            for k in range(KC):
                tp(bank_bf(b)[:, k * 128:(k + 1) * 128], xs[:, k * 128:(k + 1) * 128], ident[:],
                   ["xs", "ident"], [("ps", b)])
            tt("dve", xnT[:, :, sub * 128:(sub + 1) * 128],
               bank_bf(b).rearrange("p (k t) -> p k t", k=KC), gTbc[:], ALU.mult,
               [("ps", b), "gTbc"], ["xnT"])
            yield

    def gen_G(first):
        nbG = Rot([0, 1, 2, 3])
        bi_, bf_ = nbG(), nbG()
        for k in range(KC):
            mm(bank(bi_)[0:4, :], wif[:, k, 0:4], xnT[:, k, :], k == 0, k == KC - 1,
               ["wif", "xnT"], [("ps", bi_)])
        for k in range(KC):
            mm(bank(bf_)[0:4, :], wif[:, k, 4:8], xnT[:, k, :], k == 0, k == KC - 1,
               ["wif", "xnT"], [("ps", bf_)])
        if first:
            memset("dve", Bc[:], 0.0, ["Bc"])
            memset("dve", Gext[:], 0.0, ["Gext"])
            memset("dve", hst[:], 0.0, ["hst"])
            memset("dve", Cst[:], 0.0, [("Cst", h_) for h_ in range(H)] + ["Ctmp"])
            memset("pool", rxb[:, :, 0:3], 0.0, [("rxb", c) for c in range(KC)])
            memset("pool", mxb[:, :, 0:3], 0.0, [("mxb", c) for c in range(KC)])
        else:
            cp("dve", Gext[:, 0:1], Gext[:, 4:5], ["Gext"], ["Gext"])
        yield
        lfv, Btv, Atv, Gtv, E1v, E2v = (u32[0:4, 0, :], tr[0:4, 0, :], ti[0:4, 0, :], a_t[0:4, 0, :],
                                         a2s[0:4, 0, :], tz[0:4, 0, :])
        kl, kB, kA, kG, k1, k2 = ("u32", 0), ("tr", 0), ("ti", 0), ("a_t", 0), ("a2s", 0), ("tz", 0)
        c3 = "p (c t) -> p c t"
        act(lfv, bank(bf_)[0:4, :], AF.Exp, [("ps", bf_), "nbf"], [kl], bias=nbf[:, 0:1], scale=-1.0)
        act(lfv, lfv, AF.Ln, [kl], [kl], bias=1.0)
        yield
        scan(Btv, onesrow[:], lfv, Bc[:, 0:1], ALU.mult, ALU.subtract, ["onesrow", kl, "Bc"], [kB])
        cp("dve", Bc[:], Btv[:, T - 1:T], [kB], ["Bc"])
        stt(Atv, bank(bi_)[0:4, :], bif[:, 0:1], Btv, ALU.add, ALU.subtract, [("ps", bi_), "bif", kB], [kA])
        scan(Gtv, Atv, Atv, Gext[:, 0:1], ALU.max, ALU.max, [kA, "Gext"], [kG])
        Gv = Gtv.rearrange(c3, t=128)
        cp("dve", Gext[:, 1:5].unsqueeze(2), Gv[:, :, 127:128], [kG], ["Gext"])
        tt("dve", dl0[:], Gext[:, 0:4], Gext[:, 1:5], ALU.subtract, ["Gext"], ["dl0"])
        Rb = Gext[:, 1:5].unsqueeze(2).to_broadcast([4, 4, 128])
        tt("dve", E1v.rearrange(c3, t=128), Atv.rearrange(c3, t=128), Rb, ALU.subtract, [kA, "Gext"], [k1])
        tt("dve", E2v.rearrange(c3, t=128), Btv.rearrange(c3, t=128), Rb, ALU.add, [kB, "Gext"], [k2])
        yield
        act(dl[:], dl0[:], AF.Exp, ["dl0"], ["dl"])
        act(E1v, E1v, AF.Exp, [k1], [k1])
        act(E2v, E2v, AF.Exp, [k2], [k2], scale=-1.0)
        tt("dve", rhsd[:], dl[:].unsqueeze(2).to_broadcast([4, 4, 4]), eye4bc[:], ALU.mult,
           ["dl", "eye4bc"], ["rhsd"])
        yield
        bt_ = nbG()
        for c in range(NSUB):
            mm(bank(bt_)[:, c * 8:c * 8 + 4], E1v[:, c * 128:(c + 1) * 128], ident32[0:4, 0:4], True, True,
               [k1, "ident32"], [("ps", bt_)])
            mm(bank(bt_)[:, c * 8 + 4:c * 8 + 8], E2v[:, c * 128:(c + 1) * 128], ident32[0:4, 0:4], True, True,
               [k2, "ident32"], [("ps", bt_)])
        bd_ = nbG()
        mm(bank(bd_)[:, 0:16], ones4[:], rhsd[:].rearrange("p c h -> p (c h)"), True, True,
           ["ones4", "rhsd"], [("ps", bd_)])
        yield
        cp("dve", wtok[:].rearrange("p c j -> p (c j)"), bank(bt_)[:, 0:32], [("ps", bt_)], ["wtok"])
        cp("dve", dlt[:], bank(bd_)[:, 0:16], [("ps", bd_)], ["dlt"])
        yield

    def gen_R(pre):
        s_rx, s_rz = pre
        for m in range(KC // 2):
            if m > 0:
                s_rx = load_hu(HU_RX + m)
                s_rz = load_hu(HU_RZ + m)
            bA, bB, bC, bD = (0, 1, 2, 3) if m % 2 == 0 else (2, 3, 0, 1)
            cs = (2 * m, 2 * m + 1)
            for p, c in enumerate(cs):
                build_diag(dgR, p, PV_RCW, c, "dgR")
                b_rx = (bA, bB)[p]
                proj_fm(s_rx, p, b_rx)
                cp("dve", rxb[:, c, 3:3 + T], bank(b_rx), [("ps", b_rx)], [("rxb", c)])
            yield
            for p, c in enumerate(cs):
                b_u = (bC, bD)[p]
                for jj in range(4):
                    mm(bank(b_u), dgR[:, p, jj, :], rxb[:, c, jj:jj + T], jj == 0, jj == 3,
                       [("dgR", p), ("rxb", c)], [("ps", b_u)])
                act(ubf[:, p, :], bank(b_u), AF.Identity, [("ps", b_u), "pv"], [("ubf", p)],
                    bias=pv[:, PV_RCB, c:c + 1])
                act(u32[:, p, :], bank(b_u), AF.Identity, [("ps", b_u), "pv"], [("u32", p)],
                    bias=pv[:, PV_RCB, c:c + 1])
                cp("pool", rxb[:, c, 0:3], rxb[:, c, T:T + 3], [("rxb", c)], [("rxb", c)])
            yield
            gb = ((bA, bB), (bC, bD))
            for p, c in enumerate(cs):
                b_a, b_x = gb[p]
                mm(bank(b_a), wa[:, c, :], ubf[:, p, :], True, True, ["wa", ("ubf", p)], [("ps", b_a)])
                mm(bank(b_x), wx[:, c, :], ubf[:, p, :], True, True, ["wx", ("ubf", p)], [("ps", b_x)])
            for p, c in enumerate(cs):
                b_a, b_x = gb[p]
                act(tr[:, p, :], bank(b_a), AF.Tanh, [("ps", b_a), "hba"], [("tr", p)], bias=hba[:, c:c + 1], scale=0.5)
                act(ti[:, p, :], bank(b_x), AF.Tanh, [("ps", b_x), "hbx"], [("ti", p)], bias=hbx[:, c:c + 1], scale=0.5)
            for p, c in enumerate(cs):
                act(a_t[:, p, :], tr[:, p, :], AF.Exp, [("tr", p), "cvh"], [("a_t", p)],
                    bias=cvh[:, c:c + 1], scale=cvh[:, c:c + 1])
                act(a2s[:, p, :], tr[:, p, :], AF.Exp, [("tr", p), "cv1"], [("a2s", p)],
                    bias=cv1[:, c:c + 1], scale=cv1[:, c:c + 1])
            yield
            for p, c in enumerate(cs):
                act(a2s[:, p, :], a2s[:, p, :], AF.Sqrt, [("a2s", p)], [("a2s", p)], bias=1.0, scale=-1.0)
            for p, c in enumerate(cs):
                stt(u32[:, p, :], ti[:, p, :], 1.0, u32[:, p, :], ALU.add, ALU.mult, [("ti", p), ("u32", p)], [("u32", p)])
            yield
            for p, c in enumerate(cs):
                b_rz = (bA, bB)[p]
                proj_fm(s_rz, p, b_rz)
                act(tz[:, p, :], bank(b_rz), AF.Tanh, [("ps", b_rz)], [("tz", p)], scale=0.5)
            release(s_rx)
            release(s_rz)
            yield
            for p, c in enumerate(cs):
                tt("dve", a2s[:, p, :], a2s[:, p, :], u32[:, p, :], ALU.mult, [("a2s", p), ("u32", p)], [("a2s", p)])
                scan(hh[:, p, :], a_t[:, p, :], a2s[:, p, :], hst[:, c:c + 1], ALU.mult, ALU.add,
                     [("a_t", p), ("a2s", p), "hst"], [("hh", p)])
                cp("dve", hst[:, c:c + 1], hh[:, p, T - 1:T], [("hh", p)], ["hst"])
                yield
            for p, c in enumerate(cs):
                b_rz = (bA, bB)[p]
                stt(hh[:, p, :], bank(b_rz), 0.25, hh[:, p, :], ALU.mult, ALU.mult, [("ps", b_rz), ("hh", p)], [("hh", p)])
                stt(yrT[:, c, :], tz[:, p, :], 1.0, hh[:, p, :], ALU.add, ALU.mult, [("tz", p), ("hh", p)], [("yrT", c)])
            yield

    def gen_M(pre):
        s_mx, s_mz, s_mo = pre
        for h_ in range(H):
            if h_ > 0:
                s_mx, s_mz, s_mo = nxt_slots
            for e2 in range(2):
                c = 2 * h_ + e2
                build_diag(dgM, e2, PV_MCW, c, "dgM")
                b_mx = nbM()
                proj_fm(s_mx, e2, b_mx)
                cp("dve", mxb[:, c, 3:3 + T], bank(b_mx), [("ps", b_mx)], [("mxb", c)])
            release(s_mx)
            yield "dense"
            for e2 in range(2):
                c = 2 * h_ + e2
                b_c = nbM()
                for jj in range(4):
                    mm(bank(b_c), dgM[:, e2, jj, :], mxb[:, c, jj:jj + T], jj == 0, jj == 3,
                       [("dgM", e2), ("mxb", c)], [("ps", b_c)])
                act(c32[:, e2, :], bank(b_c), AF.Silu, [("ps", b_c), "pv"], [("t1", e2)],
                    bias=pv[:, PV_MCB, c:c + 1])
                act(cbf[:, e2, :], c32[:, e2, :], AF.Copy, [("t1", e2)], [("cbf", e2)])
                act(scm[:, e2, :], c32[:, e2, :], AF.Copy, [("t1", e2), "pv"], [("scm", e2)],
                    scale=pv[:, PV_MSK, c:c + 1])
                b_mz = nbM()
                proj_fm(s_mz, e2, b_mz)
                act(szm[:, e2, :], bank(b_mz), AF.Silu, [("ps", b_mz)], [("szm", e2)])
            release(s_mz)
            yield "dense"
            for e2 in range(2):
                b_q = nbM()
                for kc in range(2):
                    mm(bank(b_q), wq[:, h_ * 2 + kc, e2 * 128:(e2 + 1) * 128], cbf[:, kc, :], kc == 0, kc == 1,
                       ["wq", ("cbf", 0), ("cbf", 1)], [("ps", b_q)])
                cp("dve", qT[:, e2, :], bank(b_q), [("ps", b_q)], [("qT", e2)])
                b_k = nbM()
                for kc in range(2):
                    mm(bank(b_k), wk[:, h_ * 2 + kc, e2 * 128:(e2 + 1) * 128], cbf[:, kc, :], kc == 0, kc == 1,
                       ["wk", ("cbf", 0), ("cbf", 1)], [("ps", b_k)])
                ts("dve", kT[:, e2, :], bank(b_k), 1.0 / 16.0, None, ALU.mult, None, [("ps", b_k)], [("kT", e2)])
            yield "dense"
            cp("dve", wva[:, :, 256], wtok[:, :, h_], ["wtok"], ["wva"])
            for sub in range(NSUB):
                tsl = slice(sub * 128, (sub + 1) * 128)
                b_kv = nbM()
                for kc in range(2):
                    mm(bank(b_kv)[:, 0:256], cbf[:, kc, tsl], wk[:, h_ * 2 + kc, :], kc == 0, kc == 1,
                       ["wk", ("cbf", 0), ("cbf", 1)], [("ps", b_kv)])
                for kc in range(2):
                    mm(bank(b_kv)[:, 256:512], mxb[:, 2 * h_ + kc, 3 + sub * 128:3 + (sub + 1) * 128],
                       wv[:, h_ * 2 + kc, :], kc == 0, kc == 1,
                       ["wv", ("mxb", 2 * h_), ("mxb", 2 * h_ + 1)], [("ps", b_kv)])
                act(kb[:, sub, :], bank(b_kv)[:, 0:256], AF.Copy, [("ps", b_kv)], ["kb"], scale=1.0 / 16.0)
                act(wva[:, sub, 0:256], bank(b_kv)[:, 256:512], AF.Copy, [("ps", b_kv), "wtok"], ["wva"],
                    scale=wtok[:, sub, h_:h_ + 1])
                b_o = nbM()
                for k in range(KC):
                    mm(bank(b_o)[:, 0:256], xnT[:, k, tsl], ring[:, s_mo, k, :],
                       k == 0, k == KC - 1, ["xnT", ("ring", s_mo)], [("ps", b_o)])
                act(to_[:, sub, :], bank(b_o)[:, 0:256], AF.Tanh, [("ps", b_o)], [("to_", sub)], scale=0.5)
                yield "dense"
            release(s_mo)
            for e2 in range(2):
                c = 2 * h_ + e2
                cp("pool", mxb[:, c, 0:3], mxb[:, c, T:T + 3], [("mxb", c)], [("mxb", c)])
            if h_ + 1 < H:
                nxt_slots = (load_hu(HU_MX + h_ + 1), load_hu(HU_MZ + h_ + 1), load_hu(HU_MO + h_ + 1))
            for sub in range(NSUB):
                tsl = slice(sub * 128, (sub + 1) * 128)
                par = sub % 2
                hc = h_ if par == 0 else H
                hn_ = H if par == 0 else h_
                dsc = dlt[:, sub * 4 + h_:sub * 4 + h_ + 1]
                skeys = [("Cst", h_), "Ctmp"]
                act(Cbf[:, par, :, 0:257], Cst[:, hc, :, 0:257], AF.Copy, skeys + ["dlt"], [("Cbf", par)], scale=dsc)
                b_sp = nbSP()
                for kc in range(2):
                    mm(bank(b_sp)[:, 0:128], kT[:, kc, tsl], qT[:, kc, tsl], kc == 0, kc == 1,
                       [("kT", 0), ("kT", 1), ("qT", 0), ("qT", 1)], [("ps", b_sp)])
                b_dc = nbDC()
                for kc in range(2):
                    mm(bank(b_dc)[:, kc * 256:(kc + 1) * 256], kb[:, sub, kc * 128:(kc + 1) * 128],
                       wva[:, sub, 0:256], True, True, ["kb", "wva"], [("ps", b_dc)])
                yield "lat"
                tt("dve", smT[:, par, :], bank(b_sp)[:, 0:128], maskT[:], ALU.mult,
                   [("ps", b_sp), "maskT"], [("smT", par)])
                yield "lat"
                Pv = bank(b_sp)[:, 128:385]
                mm(Pv, smT[:, par, :], wva[:, sub, 0:257], True, False, [("smT", par), "wva"], [("ps", b_sp)])
                for kc in range(2):
                    mm(Pv, qT[:, kc, tsl], Cbf[:, par, kc, 0:257], False, kc == 1,
                       [("qT", 0), ("qT", 1), ("Cbf", par)], [("ps", b_sp)])
                for kc in range(2):
                    mm(bank(b_sp)[:, 400 + 2 * kc:402 + 2 * kc], kb[:, sub, kc * 128:(kc + 1) * 128],
                       wva[:, sub, 256:258], True, True, ["kb", "wva"], [("ps", b_sp)])
                yield "lat"
                stt(Cst[:, hn_, :, 0:256], Cst[:, hc, :, 0:256], dsc,
                    bank(b_dc).rearrange("p (k e) -> p k e", k=2), ALU.mult, ALU.add,
                    skeys + ["dlt", ("ps", b_dc)], skeys)
                stt(Cst[:, hn_, :, 256], Cst[:, hc, :, 256], dsc,
                    bank(b_sp)[:, 400:404].rearrange("p (k t) -> p k t", t=2)[:, :, 0], ALU.mult, ALU.add,
                    skeys + ["dlt", ("ps", b_sp)], skeys)
                stt(hmp[:, sub, :], to_[:, sub, :], 1.0, Pv[:, 0:256], ALU.add, ALU.mult,
                    [("to_", sub), ("ps", b_sp)], [("to_", sub)])
                ts("dve", den[:, sub:sub + 1], Pv[:, 256:257], -1.0, wtok[:, sub, 4 + h_:5 + h_],
                   ALU.mult, ALU.max, [("ps", b_sp), "wtok"], [("den", sub)])
                tt("dve", den[:, sub:sub + 1], den[:, sub:sub + 1], Pv[:, 256:257], ALU.max,
                   [("ps", b_sp), ("den", sub)], [("den", sub)])
                S_.add("dve", (lambda o, i: (lambda e: e.bn_stats(o, i)))(stats[:, sub, :], hmp[:, sub, :]),
                       [("to_", sub)], [("stats", sub)])
                S_.add("dve", (lambda o, i: (lambda e: e.bn_aggr(o, i)))(mv[:, sub, :], stats[:, sub, :]),
                       [("stats", sub)], [("mv", sub)])
                yield "lat"
            allsub = list(range(NSUB))
            tt("dve", epsv[:], den[:], den[:], ALU.mult, [("den", s) for s in allsub], ["epsv"])
            stt(epsv[:], epsv[:], 4.0 * EPS, mv[:, :, 1], ALU.mult, ALU.add,
                ["epsv"] + [("mv", s) for s in allsub], ["epsv"])
            tt("pool", rs4[:], epsv[:], neghalf[:], ALU.pow, ["epsv", "neghalf"], ["rs4"])
            stt(nmr[:], mv[:, :, 0], -1.0, rs4[:], ALU.mult, ALU.mult, [("mv", s) for s in allsub] + ["rs4"], ["nmr"])
            yield "lat"
            b_t = nbM()
            for sub in range(NSUB):
                act(hn[:, sub, :], hmp[:, sub, :], AF.Identity, [("to_", sub), "rs4", "nmr"], [("hn", sub)],
                    bias=nmr[:, sub:sub + 1], scale=rs4[:, sub:sub + 1])
                for e2 in range(2):
                    tp(bank_bf(b_t)[:, e2 * 512 + sub * 128:e2 * 512 + (sub + 1) * 128],
                       hn[:, sub, e2 * 128:(e2 + 1) * 128], ident[:], [("hn", sub), "ident"], [("ps", b_t)])
            yield "lat"
            for e2 in range(2):
                c = 2 * h_ + e2
                stt(t1[:, e2, :], bank_bf(b_t)[:, e2 * 512:(e2 + 1) * 512], pv[:, PV_MNG, c:c + 1], scm[:, e2, :],
                    ALU.mult, ALU.add, [("ps", b_t), "pv", ("scm", e2)], [("t1", e2)])
                tt("dve", ymT[:, c, :], t1[:, e2, :], szm[:, e2, :], ALU.mult, [("t1", e2), ("szm", e2)], [("ymT", c)])
            yield "lat"

    def gen_F(tile, tok0):
        for q in range(4):
            s_gr = load_hu(HU_GR + q)
            s_br = load_hu(HU_BR + q)
            s_gm = load_hu(HU_GM + q)
            s_bm = load_hu(HU_BM + q)
            for j in range(2):
                b_g = nb()
                proj_fm(s_gr, j, b_g)
                act(trm[:, j, :], bank(b_g), AF.Tanh, [("ps", b_g)], [("to_", 2 * j), ("to_", 2 * j + 1)], scale=0.5)
                b_r = nb()
                for k in range(KC):
                    mm(bank(b_r), lhs_hu(s_br, k, j), yrT[:, k, :], k == 0, k == KC - 1,
                       [("ring", s_br), ("yrT", k)], [("ps", b_r)])
                stt(y1[:, j, :], trm[:, j, :], 1.0, bank(b_r), ALU.add, ALU.mult,
                    [("to_", 2 * j), ("to_", 2 * j + 1), ("ps", b_r)], [("t1", j)])
            release(s_gr)
            release(s_br)
            for j in range(2):
                dc = q * 2 + j
                b_g = nb()
                proj_fm(s_gm, j, b_g)
                act(trm[:, j, :], bank(b_g), AF.Tanh, [("ps", b_g)], [("to_", 2 * j), ("to_", 2 * j + 1)], scale=0.5)
                b_m = nb()
                for k in range(KC):
                    mm(bank(b_m), lhs_hu(s_bm, k, j), ymT[:, k, :], k == 0, k == KC - 1,
                       [("ring", s_bm), ("ymT", k)], [("ps", b_m)])
                stt(y2[:], trm[:, j, :], 1.0, bank(b_m), ALU.add, ALU.mult,
                    [("to_", 2 * j), ("to_", 2 * j + 1), ("ps", b_m)], ["y2"])
                tt("dve", yT[:, dc, :], y1[:, j, :], y2[:], ALU.add, [("t1", j), "y2"], [("yT", dc)])
            release(s_gm)
            release(s_bm)
        s_o = [load_hu(HU_WO + q) for q in range(4)]
        yield
        for sub in range(NSUB):
            tsl = slice(sub * 128, (sub + 1) * 128)
            osl = sub % 2
            r0 = tok0 + sub * 128
            bo = [nb(), nb()]
            for hf in range(2):
                for q2 in range(2):
                    q = hf * 2 + q2
                    for k in range(KC):
                        mm(bank(bo[hf])[:, q2 * 256:(q2 + 1) * 256], yT[:, k, tsl], ring[:, s_o[q], k, :],
                           k == 0, k == KC - 1, [("yT", k), ("ring", s_o[q])], [("ps", bo[hf])])
                act(y2[:], bank(bo[hf]), AF.Square, [("ps", bo[hf])], ["y2", ("ss2", osl, hf)],
                    accum=ss2[:, osl, hf:hf + 1])
            tt("dve", ms2[:, osl:osl + 1], ss2[:, osl, 0:1], ss2[:, osl, 1:2], ALU.add,
               [("ss2", osl, 0), ("ss2", osl, 1)], [("ms2", osl)])
            ts("dve", ms2[:, osl:osl + 1], ms2[:, osl:osl + 1], 1.0 / D, 4.0 * EPS, ALU.mult, ALU.add,
               [("ms2", osl)], [("ms2", osl)])
            tt("pool", rs2[:, osl:osl + 1], ms2[:, osl:osl + 1], neghalf[:, 0:1], ALU.pow,
               [("ms2", osl), "neghalf"], [("rs2", osl)])
            xr = t1sz[:, 0:2, :].rearrange("p a t -> p (a t)")
            xrk = [("t1", 0), ("t1", 1)]
            dma("sp", xr, x_d[r0:r0 + 128, :], (), xrk, "xr")
            for hf in range(2):
                stt(obuf[:, hf * 512:(hf + 1) * 512], bank(bo[hf]), rs2[:, osl:osl + 1],
                    gpost_bc[:, hf * 512:(hf + 1) * 512], ALU.mult, ALU.mult,
                    [("ps", bo[hf]), ("rs2", osl), "gpost_bc"], ["obuf"])
            tt("pool", obuf[:], obuf[:], xr, ALU.add, ["obuf"] + xrk, ["obuf"])
            dma("sp", out_d[r0:r0 + 128, :], obuf[:], ["obuf"], [("out", tile, sub)], ("st", 0))
            yield
        for q in range(4):
            release(s_o[q])

    NT_ALL = NSEQ * NTILE

    def tile_tok0(tile):
        seq, tl = divmod(tile, NTILE)
        return seq * S + tl * T

    for _ in gen_X(tile_tok0(0)):
        pass
    preR = (load_hu(HU_RX), load_hu(HU_RZ))
    preM = (load_hu(HU_MX), load_hu(HU_MZ), load_hu(HU_MO))
    for tile in range(NT_ALL):
        first = tile % NTILE == 0
        tok0 = tile_tok0(tile)
        gG = gen_G(first)
        gM = gen_M(preM)
        for _ in range(3):
            next(gM)
            if late_casts:
                emit_cast(late_casts.pop(0))
            next(gG, None)
            next(gG, None)
        for _ in gG:
            pass
        gR = gen_R(preR)
        r_alive = True
        nlat = 0
        for tag in gM:
            if late_casts:
                emit_cast(late_casts.pop(0))
            if tag == "lat" and r_alive:
                nlat += 1
                if nlat % 1:
                    continue
                try:
                    next(gR)
                except StopIteration:
                    r_alive = False
        if r_alive:
            for _ in gR:
                pass
        while late_casts:
            emit_cast(late_casts.pop(0))
        gF = gen_F(tile, tok0)
        next(gF)
        if tile + 1 < NT_ALL:
            preR = (load_hu(HU_RX), load_hu(HU_RZ))
            preM = (load_hu(HU_MX), load_hu(HU_MZ), load_hu(HU_MO))
            gX = gen_X(tile_tok0(tile + 1))
            next(gX)
            for _ in gF:
                next(gX, None)
            for _ in gX:
                pass
        else:
            for _ in gF:
                pass
    S_.emit(nc, es, [k for k in [("st", 0)] if k in S_.dma_count])
    es.close()
    return nc


def prep_weights(inp):
    f = np.float32
    w_in = np.asarray(inp["w_in"][0], f)
    cols = {}
    offs = [0, 1024, 2048, 3072, 4096, 5120, 5124, 5128, 6152, 7176]
    names = ["r_x", "r_z", "m_x", "m_z", "m_o", "m_i", "m_f", "gate_r", "gate_m"]
    for n, a, b in zip(names, offs[:-1], offs[1:]):
        cols[n] = w_in[:, a:b]
    big = [cols["r_x"], cols["r_z"], cols["m_x"], cols["m_z"], cols["m_o"], cols["gate_r"], cols["gate_m"],
           np.asarray(inp["w_branch_r"][0], f), np.asarray(inp["w_branch_m"][0], f), np.asarray(inp["w_out"][0], f)]
    units = []
    for W in big:
        for q in range(4):
            Wh = W[:, q * 256:(q + 1) * 256]
            units.append(Wh.reshape(8, 128, 256).transpose(1, 0, 2))
    wbig = np.ascontiguousarray(np.stack(units, 0))
    qkv = []
    for n in ("ml_w_q", "ml_w_k", "ml_w_v"):
        W = np.asarray(inp[n][0], f)
        qkv.append(W.reshape(4, 2, 128, 256).transpose(2, 0, 1, 3).reshape(128, 8, 256))
    wqkv = np.ascontiguousarray(np.stack(qkv, 0))
    gates = []
    for n in ("rg_w_a", "rg_w_x"):
        W = np.asarray(inp[n][0], f)
        G = np.zeros((128, 8, 128), f)
        for c in range(8):
            G[0:64, c, 0:64] = W[2 * c]
            G[64:128, c, 64:128] = W[2 * c + 1]
        gates.append(G)
    wgate = np.ascontiguousarray(np.stack(gates, 0))
    wif = np.ascontiguousarray(w_in[:, 5120:5128].reshape(8, 128, 8).transpose(1, 0, 2))
    pnames = ["g_pre", "rg_conv_b", "rg_b_a", "rg_b_x", "rg_lambda", "ml_conv_b", "ml_norm_g", "ml_skip"]
    prow = [np.asarray(inp[n][0], f) for n in pnames]
    prow += [np.asarray(inp["rg_conv_w"][0], f)[j] for j in range(4)]
    prow += [np.asarray(inp["ml_conv_w"][0], f)[j] for j in range(4)]
    pvec = np.ascontiguousarray(np.stack([r.reshape(8, 128).T for r in prow], 1))
    bif = np.ascontiguousarray(np.stack([np.asarray(inp["ml_b_i"][0], f), np.asarray(inp["ml_b_f"][0], f)], 1))
    gpost = np.ascontiguousarray(np.asarray(inp["g_post"][0], f).reshape(1, 1024))
    c_ident = np.eye(128, dtype=f)
    c_mask = np.triu(np.ones((128, 128), f))
    return dict(wbig=wbig, wqkv=wqkv, wgate=wgate, wif=wif, pvec=pvec, bif=bif, gpost=gpost,
                c_ident=c_ident, c_mask=c_mask)


_NC_CACHE = {}


def kernel(**inputs):
    x = np.asarray(inputs["x"], np.float32)
    B, S, _ = x.shape
    ncores = 8
    nseq = B // ncores
    key = (nseq, S)
    if key not in _NC_CACHE:
        _NC_CACHE[key] = build(nseq, S)
    nc = _NC_CACHE[key]
    wts = prep_weights(inputs)
    in_maps = []
    for c in range(ncores):
        m = dict(wts)
        m["x"] = np.ascontiguousarray(x[c * nseq:(c + 1) * nseq].reshape(nseq * S, D))
        in_maps.append(m)
    res = run_bass_kernel_spmd(nc, in_maps, core_ids=list(range(ncores)))
    outs = [np.asarray(r["out"], np.float32).reshape(nseq, S, D) for r in res.results]
    return np.concatenate(outs, 0)
```
